# Optimizing a Trainium2 kernel written in Bass

```python
import math
import jax, jax.numpy as jnp
from jax import lax
import numpy as np

D_MODEL = 1024
BATCH = 8
SEQ = 2048
DEPTH = 1

ATT_HEADS = 16
ATT_HEAD_DIM = 64
KV_LATENT = 128
IDX_HEADS = 8
IDX_DIM = 64
TOPK_MAX = 256
Q_BLOCK = 128
REL_BUCKETS = 32
REL_MAX_DIST = 128
SSM_INNER = 2 * D_MODEL
SSM_HEAD_DIM = 64
SSM_HEADS = SSM_INNER // SSM_HEAD_DIM
SSM_GROUPS = 4
SSM_HEADS_PER_GROUP = SSM_HEADS // SSM_GROUPS
SSM_STATE = 128
SSM_CONV = 4
SSM_CHUNK = 128
SSM_XBC = SSM_INNER + 2 * SSM_GROUPS * SSM_STATE
FFN_DIM = 2816
FFN_CONV = 3
EPS = 1e-6

IN_SPLITS = (
    ATT_HEADS * ATT_HEAD_DIM,
    KV_LATENT,
    IDX_HEADS * IDX_DIM,
    IDX_DIM,
    IDX_HEADS,
    SSM_INNER,
    SSM_XBC,
    SSM_HEADS,
    D_MODEL,
    D_MODEL,
)
IN_COLS = sum(IN_SPLITS)

kernel_name = "hybrid_dsa_ssd_convffn_block"


def split_cols(a, sizes):
    idx, acc = [], 0
    for sz in sizes[:-1]:
        acc += sz
        idx.append(acc)
    return jnp.split(a, idx, axis=-1)


def rmsnorm(x, g):
    x32 = x.astype(jnp.float32)
    y = x32 * lax.rsqrt(jnp.mean(x32 * x32, axis=-1, keepdims=True) + EPS)
    return (y * g.astype(jnp.float32)).astype(x.dtype)


def causal_depthwise_conv(u, w, bias):
    k, ch = w.shape
    out = lax.conv_general_dilated(
        u, w[:, None, :].astype(u.dtype), window_strides=(1,), padding=[(k - 1, 0)],
        dimension_numbers=("NWC", "WIO", "NWC"), feature_group_count=ch)
    return out + bias.astype(u.dtype)


def t5_bucket(dist):
    n = jnp.maximum(dist, 0)
    max_exact = REL_BUCKETS // 2
    large = max_exact + (jnp.log(jnp.maximum(n, 1).astype(jnp.float32) / max_exact)
                         / math.log(REL_MAX_DIST / max_exact)
                         * (REL_BUCKETS - max_exact)).astype(jnp.int32)
    large = jnp.minimum(large, REL_BUCKETS - 1)
    return jnp.where(n < max_exact, n, large)


def dsa_attention(q_lat, c_kv, q_idx, k_idx, w_idx, rel_bias):
    b, s, h, c = q_lat.shape
    n_sel = min(TOPK_MAX, s // 4)
    nblk = s // Q_BLOCK
    scale = ATT_HEAD_DIM ** -0.5
    key_pos = jnp.arange(s)
    gather = jax.vmap(lambda table, idx: table[idx])

    def to_blocks(a):
        return a.reshape(b, nblk, Q_BLOCK, *a.shape[2:]).swapaxes(0, 1)

    def block(args):
        qb, qib, wib, blk = args
        q_pos = blk * Q_BLOCK + jnp.arange(Q_BLOCK)
        causal = key_pos[None, :] <= q_pos[:, None]
        idx_logits = jnp.einsum("bqhd,bsd->bqhs", qib, k_idx,
                                preferred_element_type=jnp.float32)
        score = jnp.einsum("bqhs,bqh->bqs", jax.nn.relu(idx_logits),
                           wib.astype(jnp.float32))
        score = jnp.where(causal[None], score, -jnp.inf)
        _, sel = lax.top_k(score, n_sel)
        c_sel = gather(c_kv, sel)
        logits = jnp.einsum("bqhc,bqkc->bhqk", qb, c_sel,
                            preferred_element_type=jnp.float32) * scale
        dist = q_pos[None, :, None] - sel
        bias = rel_bias[t5_bucket(dist)]
        logits = logits + jnp.moveaxis(bias, -1, 1).astype(jnp.float32)
        logits = jnp.where((dist >= 0)[:, None], logits, -jnp.inf)
        p = jax.nn.softmax(logits, axis=-1).astype(c_sel.dtype)
        return jnp.einsum("bhqk,bqkc->bqhc", p, c_sel)

    out = lax.map(block, (to_blocks(q_lat), to_blocks(q_idx), to_blocks(w_idx),
                          jnp.arange(nblk)))
    return out.swapaxes(0, 1).reshape(b, s, h, c)


def ssd_scan(xh, dt, a, bmat, cmat):
    b, l, g, r, p = xh.shape
    n = bmat.shape[-1]
    q = SSM_CHUNK
    nc = l // q
    f32 = jnp.float32
    x = (xh.astype(f32) * dt[..., None]).reshape(b, nc, q, g, r, p)
    adt = (dt * a).reshape(b, nc, q, g, r)
    bm = bmat.astype(f32).reshape(b, nc, q, g, n)
    cm = cmat.astype(f32).reshape(b, nc, q, g, n)
    a_cum = jnp.cumsum(adt, axis=2)
    tri = jnp.tril(jnp.ones((q, q), dtype=bool))
    seg = a_cum[:, :, :, None] - a_cum[:, :, None, :]
    decay = jnp.exp(jnp.where(tri[None, None, :, :, None, None], seg, -jnp.inf))
    cb = jnp.einsum("bclgn,bcsgn->bclsg", cm, bm)
    y_diag = jnp.einsum("bclsgr,bcsgrp->bclgrp", cb[..., None] * decay, x)
    decay_states = jnp.exp(a_cum[:, :, -1:] - a_cum)
    states = jnp.einsum("bclgn,bclgrp->bcgrpn", bm, x * decay_states[..., None])
    chunk_decay = jnp.exp(a_cum[:, :, -1])

    def step(hstate, inp):
        st, dec = inp
        return hstate * dec[..., None, None] + st, hstate

    h0 = jnp.zeros((b, g, r, p, n), f32)
    _, prev = lax.scan(step, h0, (states.swapaxes(0, 1), chunk_decay.swapaxes(0, 1)))
    prev = prev.swapaxes(0, 1)
    y_off = jnp.einsum("bclgn,bcgrpn->bclgrp", cm, prev) * jnp.exp(a_cum)[..., None]
    return (y_diag + y_off).reshape(b, l, g, r, p)


def gated_group_rmsnorm(y, z, g):
    u = y.astype(jnp.float32) * jax.nn.silu(z.astype(jnp.float32))
    b, l, c = u.shape
    u = u.reshape(b, l, SSM_GROUPS, c // SSM_GROUPS)
    u = u * lax.rsqrt(jnp.mean(u * u, axis=-1, keepdims=True) + EPS)
    return (u.reshape(b, l, c) * g.astype(jnp.float32)).astype(z.dtype)


def setup_inputs(seed: int = 0) -> dict:
    key = jax.random.key(seed)
    ks = jax.random.split(key, 24)
    f32 = jnp.float32

    def nrm(k, shape, scale):
        return jax.random.normal(k, shape, f32) * scale

    def gain(k, shape):
        return 1.0 + 0.05 * jax.random.normal(k, shape, f32)

    dt0 = jnp.exp(jax.random.uniform(ks[8], (DEPTH, SSM_HEADS), f32,
                                     math.log(1e-3), math.log(1e-1)))
    dt_bias = dt0 + jnp.log(-jnp.expm1(-dt0))
    return {
        "x": nrm(ks[0], (BATCH, SEQ, D_MODEL), 1.0),
        "rel_bias": nrm(ks[1], (REL_BUCKETS, ATT_HEADS), 0.5),
        "norm_mix": gain(ks[2], (DEPTH, D_MODEL)),
        "w_in": nrm(ks[3], (DEPTH, D_MODEL, IN_COLS), D_MODEL ** -0.5),
        "kv_norm": gain(ks[4], (DEPTH, KV_LATENT)),
        "w_uk": nrm(ks[5], (DEPTH, ATT_HEADS, ATT_HEAD_DIM, KV_LATENT), ATT_HEAD_DIM ** -0.5),
        "w_uv": nrm(ks[6], (DEPTH, ATT_HEADS, ATT_HEAD_DIM, KV_LATENT), KV_LATENT ** -0.5),
        "conv_ssm_w": nrm(ks[7], (DEPTH, SSM_CONV, SSM_XBC), SSM_CONV ** -0.5),
        "conv_ssm_b": nrm(ks[9], (DEPTH, SSM_XBC), 0.01),
        "dt_bias": dt_bias,
        "a_log": jnp.log(jax.random.uniform(ks[10], (DEPTH, SSM_HEADS), f32, 1.0, 16.0)),
        "d_skip": 1.0 + 0.1 * jax.random.normal(ks[11], (DEPTH, SSM_HEADS), f32),
        "ssm_norm": gain(ks[12], (DEPTH, SSM_INNER)),
        "w_att_out": nrm(ks[13], (DEPTH, ATT_HEADS * ATT_HEAD_DIM, D_MODEL),
                         (ATT_HEADS * ATT_HEAD_DIM) ** -0.5),
        "w_ssm_out": nrm(ks[14], (DEPTH, SSM_INNER, D_MODEL), SSM_INNER ** -0.5),
        "w_out": nrm(ks[15], (DEPTH, D_MODEL, D_MODEL), D_MODEL ** -0.5),
        "norm_ffn": gain(ks[16], (DEPTH, D_MODEL)),
        "w_ffn_up": nrm(ks[17], (DEPTH, D_MODEL, 2 * FFN_DIM), D_MODEL ** -0.5),
        "conv_ffn_w": nrm(ks[18], (DEPTH, FFN_CONV, 2 * FFN_DIM), FFN_CONV ** -0.5),
        "conv_ffn_b": nrm(ks[19], (DEPTH, 2 * FFN_DIM), 0.01),
        "w_ffn_down": nrm(ks[20], (DEPTH, FFN_DIM, D_MODEL), FFN_DIM ** -0.5),
        "norm_final": gain(ks[21], (D_MODEL,)),
    }


def reference(x, rel_bias, norm_mix, w_in, kv_norm, w_uk, w_uv, conv_ssm_w, conv_ssm_b,
              dt_bias, a_log, d_skip, ssm_norm, w_att_out, w_ssm_out, w_out, norm_ffn,
              w_ffn_up, conv_ffn_w, conv_ffn_b, w_ffn_down, norm_final):
    b, s, _ = x.shape
    for layer in range(DEPTH):
        h = rmsnorm(x, norm_mix[layer])
        proj = h @ w_in[layer]
        (q, c_raw, q_idx, k_idx, w_idx, z, xbc, dt_raw, g_att, g_ssm) = split_cols(proj, IN_SPLITS)

        q = q.reshape(b, s, ATT_HEADS, ATT_HEAD_DIM)
        q_lat = jnp.einsum("bshd,hdc->bshc", q, w_uk[layer])
        c_kv = rmsnorm(c_raw, kv_norm[layer])
        att_lat = dsa_attention(q_lat, c_kv,
                                q_idx.reshape(b, s, IDX_HEADS, IDX_DIM), k_idx, w_idx,
                                rel_bias)
        att = jnp.einsum("bshc,hdc->bshd", att_lat, w_uv[layer])
        y_att = att.reshape(b, s, ATT_HEADS * ATT_HEAD_DIM) @ w_att_out[layer]

        xbc = jax.nn.silu(causal_depthwise_conv(xbc, conv_ssm_w[layer], conv_ssm_b[layer]))
        xs, bm, cm = split_cols(xbc, (SSM_INNER, SSM_GROUPS * SSM_STATE, SSM_GROUPS * SSM_STATE))
        dt = jax.nn.softplus(dt_raw.astype(jnp.float32) + dt_bias[layer].astype(jnp.float32))
        a = -jnp.exp(a_log[layer].astype(jnp.float32))
        xh = xs.reshape(b, s, SSM_GROUPS, SSM_HEADS_PER_GROUP, SSM_HEAD_DIM)
        y = ssd_scan(xh,
                     dt.reshape(b, s, SSM_GROUPS, SSM_HEADS_PER_GROUP),
                     a.reshape(SSM_GROUPS, SSM_HEADS_PER_GROUP),
                     bm.reshape(b, s, SSM_GROUPS, SSM_STATE),
                     cm.reshape(b, s, SSM_GROUPS, SSM_STATE))
        y = y + d_skip[layer].astype(jnp.float32).reshape(
            SSM_GROUPS, SSM_HEADS_PER_GROUP)[..., None] * xh.astype(jnp.float32)
        y = gated_group_rmsnorm(y.reshape(b, s, SSM_INNER), z, ssm_norm[layer])
        y_ssm = y @ w_ssm_out[layer]

        merged = jax.nn.sigmoid(g_att) * y_att + jax.nn.sigmoid(g_ssm) * y_ssm
        x = x + merged @ w_out[layer]

        h = rmsnorm(x, norm_ffn[layer])
        u = causal_depthwise_conv(h @ w_ffn_up[layer], conv_ffn_w[layer], conv_ffn_b[layer])
        gate, val = jnp.split(u, 2, axis=-1)
        x = x + (jax.nn.silu(gate) * val) @ w_ffn_down[layer]
    return rmsnorm(x, norm_final)
```

```python
import numpy as np
import concourse.bass as bass
import concourse.mybir as mybir

F32 = mybir.dt.float32
BF16 = mybir.dt.bfloat16
AF = mybir.ActivationFunctionType
ALU = mybir.AluOpType
AX = mybir.AxisListType

SCHED_LOG = None
ENGS = ("pe", "dve", "act", "pool", "sp")
_ESZ = {F32: 4, BF16: 2}


def _esize(dt):
    return _ESZ[dt]


class _Op:
    __slots__ = ("eng", "fn", "deps", "needs_inc", "sig", "is_dma", "dsem", "dcount", "prev_dma", "idx")

    def __init__(self, eng, fn, is_dma=False):
        self.eng = eng
        self.fn = fn
        self.deps = []
        self.needs_inc = False
        self.sig = 0
        self.is_dma = is_dma
        self.dsem = None
        self.dcount = 0
        self.prev_dma = None


class Prog:
    NDMA = {"sp": 10, "pool": 6, "act": 4}

    def __init__(self, nc):
        self.nc = nc
        self.ops = {e: [] for e in ENGS}
        self.recs = {}
        self.dma_rr = {e: 0 for e in self.NDMA}
        self.dma_last = {}
        self.dma_cnt = {}
        self.nops = 0

    def region(self, ap):
        t = ap.tensor
        name = t.name
        es = _esize(ap.dtype)
        dims = ap.ap
        if name in ("arena", "psum"):
            pstride = dims[0][0]
            npart = dims[0][1]
            p0 = ap.offset // pstride
            rem = ap.offset % pstride
            ext = 0
            for st, cnt in dims[1:]:
                ext += (cnt - 1) * abs(st)
            if name == "psum":
                lo = (rem * es) // 2048 * 2048
                hi = ((rem + ext + 1) * es + 2047) // 2048 * 2048
                return ("ps", 0, 128, lo, hi)
            return ("sb", p0, p0 + npart, rem * es, (rem + ext + 1) * es)
        ext = 0
        for st, cnt in dims:
            ext += (cnt - 1) * abs(st)
        return ("d:" + name, 0, 1, ap.offset * es, (ap.offset + ext + 1) * es)

    def _track(self, op, reads, writes):
        deps = set()
        for ap in reads:
            sp, p0, p1, b0, b1 = self.region(ap)
            lst = self.recs.setdefault(sp, [])
            for r in lst:
                if r[5] == "w" and r[0] < p1 and p0 < r[1] and r[2] < b1 and b0 < r[3]:
                    deps.add((r[4], "raw"))
        for ap in writes:
            sp, p0, p1, b0, b1 = self.region(ap)
            lst = self.recs.setdefault(sp, [])
            for r in lst:
                if r[0] < p1 and p0 < r[1] and r[2] < b1 and b0 < r[3]:
                    deps.add((r[4], "raw" if False else ("waw" if r[5] == "w" else "war")))
        for ap in reads:
            sp, p0, p1, b0, b1 = self.region(ap)
            lst = self.recs[sp]
            found = False
            for r in lst:
                if r[5] == "r" and r[4].eng == op.eng and r[0] == p0 and r[1] == p1 and r[2] == b0 and r[3] == b1:
                    r[4] = op
                    found = True
                    break
            if not found:
                lst.append([p0, p1, b0, b1, op, "r"])
        for ap in writes:
            sp, p0, p1, b0, b1 = self.region(ap)
            lst = self.recs[sp]
            lst[:] = [r for r in lst if not (r[0] >= p0 and r[1] <= p1 and r[2] >= b0 and r[3] <= b1)]
            lst.append([p0, p1, b0, b1, op, "w"])
        best = {}
        for prod, kind in deps:
            if prod is op:
                continue
            if prod.eng == op.eng and not prod.is_dma and not op.is_dma:
                if op.eng == "pe":
                    continue
            key = prod.dsem if prod.is_dma else prod.eng
            cur = best.get(key)
            if cur is None or prod.idx > cur.idx:
                best[key] = prod
        for prod in best.values():
            op.deps.append(prod)
            prod.needs_inc = True

    def add(self, eng, fn, reads=(), writes=()):
        op = _Op(eng, fn)
        op.idx = len(self.ops[eng])
        self.ops[eng].append(op)
        self._track(op, reads, writes)
        self.nops += 1
        return op

    def dma(self, eng, out, in_, **kw):
        op = _Op(eng, None, is_dma=True)
        j = self.dma_rr[eng]
        self.dma_rr[eng] = (j + 1) % self.NDMA[eng]
        key = (eng, j)
        op.dsem = key
        op.prev_dma = self.dma_last.get(key)
        op.dcount = self.dma_cnt.get(key, 0) + 1
        self.dma_cnt[key] = op.dcount
        self.dma_last[key] = op
        op.fn = lambda e: e.dma_start(out=out, in_=in_, **kw)
        op.idx = len(self.ops[eng])
        self.ops[eng].append(op)
        self._track(op, [in_], [out])
        self.nops += 1
        return op

    def wait_for(self, eng, prods):
        op = _Op(eng, None)
        op.idx = len(self.ops[eng])
        for p in prods:
            op.deps.append(p)
            p.needs_inc = True
        self.ops[eng].append(op)
        return op

    def emit(self):
        nc = self.nc
        for e in ENGS:
            c = 0
            for op in self.ops[e]:
                if op.is_dma:
                    continue
                if op.needs_inc:
                    c += 1
                    op.sig = c
        import contextlib
        with contextlib.ExitStack() as st:
            esem = {e: st.enter_context(nc.semaphore("s_" + e)) for e in ENGS}
            dsem = {}
            for e, n in self.NDMA.items():
                for j in range(n):
                    dsem[(e, j)] = st.enter_context(nc.semaphore("d_%s%d" % (e, j)))
            block = st.enter_context(nc.Block())
            engobj = {"pe": block.tensor, "dve": block.vector, "act": block.scalar, "pool": block.gpsimd, "sp": block.sync}

            def make(e):
                def body(eng):
                    waited = {}
                    for op in self.ops[e]:
                        need = []
                        for p in op.deps:
                            if p.is_dma:
                                need.append((("d",) + p.dsem, dsem[p.dsem], 16 * p.dcount))
                            else:
                                need.append((("e", p.eng), esem[p.eng], p.sig))
                        if op.is_dma and op.prev_dma is not None:
                            p = op.prev_dma
                            need.append((("d",) + p.dsem, dsem[p.dsem], 16 * p.dcount))
                        for key, sem, val in need:
                            if waited.get(key, 0) >= val:
                                continue
                            waited[key] = val
                            eng.wait_ge(sem, val)
                            if SCHED_LOG is not None:
                                SCHED_LOG.append("%s wait %s >= %d" % (e, key, val))
                        if SCHED_LOG is not None:
                            SCHED_LOG.append("%s op#%d %s inc=%s" % (e, op.idx, "dma" if op.is_dma else ("wait" if op.fn is None else "op"),
                                             (op.dsem if op.is_dma else (op.sig if op.needs_inc else None))))
                        if op.fn is None:
                            continue
                        ins = op.fn(eng)
                        if op.is_dma:
                            ins.then_inc(dsem[op.dsem], 16)
                        elif op.needs_inc:
                            ins.then_inc(esem[e], 1)
                return body

            for e in ENGS:
                engobj[e](make(e))


class Arena:
    def __init__(self, nc, st, kb=200):
        self.t = st.enter_context(nc.sbuf_tensor("arena", [128, kb * 256], F32))
        self.ps = st.enter_context(nc.psum_tensor("psum", [128, 4096], F32))
        self.top = 0
        self.cap = kb * 1024
        self.peak = 0

    def alloc(self, shape, dtype, top=False):
        es = _esize(dtype)
        n = 1
        for s in shape:
            n *= s
        nb = (n * es + 63) // 64 * 64
        if top:
            self.cap -= nb
            off = self.cap
        else:
            off = self.top
            self.top += nb
        self.peak = max(self.peak, self.top)
        assert self.top <= self.cap, "SBUF arena overflow %d > %d" % (self.top, self.cap)
        v = self.t[:, off // 4:(off + nb) // 4].bitcast(dtype)[:, 0:n]
        return _reshape(v, shape)

    def mark(self):
        return self.top

    def release(self, m):
        self.top = m

    def release_top(self, kb=200):
        self.cap = kb * 1024

    def psum(self, off_f32, shape, dtype=F32):
        es = _esize(dtype)
        n = 1
        for s in shape:
            n *= s
        nw = (n * es + 3) // 4
        v = self.ps[:, off_f32:off_f32 + nw]
        if dtype != F32:
            v = v.bitcast(dtype)[:, 0:n]
        return _reshape(v, shape)


def _reshape(v, shape):
    if len(shape) == 1:
        return v
    names = "abcdefg"[:len(shape)]
    pat = "p (%s) -> p %s" % (" ".join(names), " ".join(names))
    kw = {names[k]: shape[k] for k in range(len(shape))}
    return v.rearrange(pat, **kw)

from concourse.bass_utils import run_bass_kernel_spmd
import contextlib

S = 2048
DM = 1024
NT = 16
EPS = 1e-6
OQ, OC, OQI, OKI, OWI, OZ, OXBC, ODT, OGA, OGS = 0, 1024, 1152, 1664, 1728, 1736, 3784, 6856, 6888, 7912
NIT = 24
C_GMIX, C_GFFN, C_GSSM, C_CWS, C_CBS, C_CWF, C_CBF = 0, 8, 16, 32, 128, 152, 284
C_DTB, C_ALOG, C_DSK, C_KVG, C_B31, C_GFIN, C_ID32, C_NTRI, C_PW = 328, 360, 392, 424, 552, 568, 1592, 1720, 1848
C_GMIXB = C_PW + NIT
C_GFFNB = C_GMIXB + 1024
NCONST = C_GFFNB + 1024
B_ID, B_T1, B_T2, B_ONE, B_NEG1, B_NM8 = 0, 128, 256, 384, 512, 640
NCBF = B_NM8 + 1024


def _t5_bucket(n):
    n = np.maximum(n, 0)
    max_exact = 16
    large = max_exact + (np.log(np.maximum(n, 1).astype(np.float32) / max_exact)
                         / np.float32(np.log(128 / max_exact)) * (32 - max_exact)).astype(np.int32)
    large = np.minimum(large, 31)
    return np.where(n < max_exact, n, large)


def _prep_shared(inp):
    f = np.float32
    w_in = np.asarray(inp["w_in"], f)[0]

    def pk(cols):
        a = w_in[:, cols]
        return np.ascontiguousarray(a.reshape(8, 128, a.shape[1]).transpose(1, 0, 2))

    def rows_pk(w, nk):
        return np.ascontiguousarray(w.reshape(nk, 128, w.shape[1]).transpose(1, 0, 2))

    d = {}
    d["wq"] = pk(np.arange(OQ, OQ + 1024))
    d["wqi"] = pk(np.arange(OQI, OQI + 512))
    d["wki2"] = pk(np.concatenate([np.arange(OKI, OKI + 64)] * 2))
    d["wtok"] = pk(np.concatenate([np.arange(OC, OC + 128), np.arange(OWI, OWI + 8)]))
    d["wdt"] = pk(np.arange(ODT, ODT + 32))
    d["wga"] = np.stack([pk(np.arange(OGA + n * 128, OGA + (n + 1) * 128)) for n in range(8)])
    d["wgs"] = np.stack([pk(np.arange(OGS + n * 128, OGS + (n + 1) * 128)) for n in range(8)])
    ws = []
    for g in range(4):
        cols = np.concatenate([np.arange(OZ + g * 512, OZ + (g + 1) * 512),
                               np.arange(OXBC + g * 512, OXBC + (g + 1) * 512),
                               np.arange(OXBC + 2048 + g * 128, OXBC + 2048 + (g + 1) * 128),
                               np.arange(OXBC + 2560 + g * 128, OXBC + 2560 + (g + 1) * 128)])
        ws.append(pk(cols))
    d["wssm"] = np.stack(ws)
    wso = np.asarray(inp["w_ssm_out"], f)[0]
    d["wso"] = np.stack([rows_pk(wso[:, n * 128:(n + 1) * 128], 16) for n in range(8)])
    wao = np.asarray(inp["w_att_out"], f)[0]
    d["wao"] = np.stack([rows_pk(wao[:, n * 128:(n + 1) * 128], 8) for n in range(8)])
    d["wout"] = rows_pk(np.asarray(inp["w_out"], f)[0], 8)
    wup = np.asarray(inp["w_ffn_up"], f)[0]
    d["wup"] = np.stack([rows_pk(np.concatenate([wup[:, c * 128:(c + 1) * 128],
                                                 wup[:, 2816 + c * 128:2816 + (c + 1) * 128]], axis=1), 8)
                         for c in range(22)])
    d["wdn"] = rows_pk(np.asarray(inp["w_ffn_down"], f)[0], 22)
    wuk = np.asarray(inp["w_uk"], f)[0]
    d["wuk"] = np.ascontiguousarray(wuk.reshape(8, 2, 64, 128).transpose(1, 2, 0, 3).reshape(128, 8, 128))
    wuv = np.asarray(inp["w_uv"], f)[0]
    d["wuvT"] = np.ascontiguousarray(wuv.transpose(2, 0, 1))

    cst = np.zeros((128, NCONST), f)

    def pp(v, nk):
        return np.asarray(v, f).reshape(nk, 128).T

    def bc(v):
        return np.broadcast_to(np.asarray(v, f).reshape(1, -1), (128, np.asarray(v).size))

    cst[:, C_GMIX:C_GMIX + 8] = pp(inp["norm_mix"][0], 8)
    cst[:, C_GFFN:C_GFFN + 8] = pp(inp["norm_ffn"][0], 8)
    cst[:, C_GSSM:C_GSSM + 16] = pp(inp["ssm_norm"][0], 16)
    cws = np.asarray(inp["conv_ssm_w"], f)[0]
    cst[:, C_CWS:C_CWS + 96] = cws.T.reshape(24, 128, 4).transpose(1, 0, 2).reshape(128, 96)
    cst[:, C_CBS:C_CBS + 24] = pp(inp["conv_ssm_b"][0], 24)
    cwf = np.asarray(inp["conv_ffn_w"], f)[0]
    cst[:, C_CWF:C_CWF + 132] = cwf.T.reshape(44, 128, 3).transpose(1, 0, 2).reshape(128, 132)
    cst[:, C_CBF:C_CBF + 44] = pp(inp["conv_ffn_b"][0], 44)
    cst[:, C_DTB:C_DTB + 32] = bc(inp["dt_bias"][0])
    cst[:, C_ALOG:C_ALOG + 32] = bc(inp["a_log"][0])
    cst[:, C_DSK:C_DSK + 32] = bc(inp["d_skip"][0])
    cst[:, C_KVG:C_KVG + 128] = bc(inp["kv_norm"][0])
    rb = np.asarray(inp["rel_bias"], f)
    cst[:, C_B31:C_B31 + 16] = bc(rb[31])
    cst[:, C_GFIN:C_GFIN + 1024] = bc(inp["norm_final"])
    r = np.arange(128)
    cst[:, C_ID32:C_ID32 + 128] = np.eye(128, dtype=f)
    cst[:, C_NTRI:C_NTRI + 128] = np.where(r[:, None] < r[None, :], f(-1e30), f(0))
    cst[:, C_PW:C_PW + NIT] = (0.5 ** np.arange(1, NIT + 1)).astype(f)[None, :]
    cst[:, C_GMIXB:C_GMIXB + 1024] = bc(inp["norm_mix"][0])
    cst[:, C_GFFNB:C_GFFNB + 1024] = bc(inp["norm_ffn"][0])
    d["consts"] = cst
    cb = np.zeros((128, NCBF), f)
    cb[:, B_ID:B_ID + 128] = np.eye(128)
    cb[:, B_T1:B_T1 + 128] = (r[:, None] <= r[None, :])
    cb[:, B_T2:B_T2 + 128] = (r[:, None] > r[None, :])
    cb[:, B_ONE:B_ONE + 128] = 1.0
    cb[:, B_NEG1:B_NEG1 + 128] = -1.0
    cb[:, B_NM8:B_NM8 + 1024] = np.tile(np.where(r[:, None] > r[None, :], f(-30000.0), f(0)), (1, 8))
    d["cbf"] = cb
    bt = np.zeros((128, 2, 16, 128), f)
    for dlt in range(2):
        dist = 128 * dlt + r[None, :] - r[:, None]
        bk = _t5_bucket(dist)
        g = rb[bk]
        bt[:, dlt] = g.transpose(0, 2, 1)
    d["biasT"] = bt
    return d


SHAPES = {"wq": [128, 8, 1024], "wqi": [128, 8, 512], "wki2": [128, 8, 128], "wtok": [128, 8, 136],
          "wdt": [128, 8, 32], "wga": [8, 128, 8, 128], "wgs": [8, 128, 8, 128], "wssm": [4, 128, 8, 1280],
          "wso": [8, 128, 16, 128], "wao": [8, 128, 8, 128], "wout": [128, 8, 1024], "wup": [22, 128, 8, 256],
          "wdn": [128, 22, 1024], "wuk": [128, 8, 128], "wuvT": [128, 16, 64], "consts": [128, NCONST],
          "cbf": [128, NCBF], "biasT": [128, 2, 16, 128]}


def build(debug=(), stop=None):
    nc = bass.Bass("TRN2", target_bir_lowering=False)
    D = {}
    for k, shp in SHAPES.items():
        D[k] = nc.dram_tensor(k, shp, F32, kind="ExternalInput").ap()
    xd = nc.dram_tensor("x", [S, DM], F32, kind="ExternalInput").ap()
    outd = nc.dram_tensor("out", [S, DM], F32, kind="ExternalOutput").ap()
    x1d = nc.dram_tensor("x1d", [S, DM], F32, kind="Internal").ap()
    dbg = {}
    if "hT" in debug:
        dbg["hT"] = nc.dram_tensor("dbg_hT", [128, 8 * S], F32, kind="ExternalOutput").ap()
    if "ynT" in debug:
        dbg["ynT"] = nc.dram_tensor("dbg_ynT", [128, 16 * S], F32, kind="ExternalOutput").ap()
    if "m1" in debug:
        dbg["m1"] = nc.dram_tensor("dbg_m1", [128, 8 * S], F32, kind="ExternalOutput").ap()
    if "m2" in debug:
        dbg["m2"] = nc.dram_tensor("dbg_m2", [128, 8 * S], F32, kind="ExternalOutput").ap()

    with contextlib.ExitStack() as st:
        A = Arena(nc, st, kb=200)
        P = Prog(nc)
        st_flip = [0]

        def isap(v):
            return not isinstance(v, (int, float)) and v is not None

        def mm(out, lhsT, rhs, start=True, stop=True, **kw):
            rd = [lhsT, rhs] + ([] if start else [out])
            return P.add("pe", lambda e: e.matmul(out, lhsT=lhsT, rhs=rhs, start=start, stop=stop, **kw), rd, [out])

        def tr(out, in_, ident):
            return P.add("pe", lambda e: e.transpose(out, in_, ident), [in_, ident], [out])

        def act(out, in_, func, bias=None, scale=1.0, accum=None):
            rd = [in_] + [v for v in (bias, scale) if isap(v)]
            wr = [out] + ([accum] if accum is not None else [])
            kw = {}
            if bias is not None:
                kw["bias"] = bias
            if accum is not None:
                kw["accum_out"] = accum
            return P.add("act", lambda e: e.activation(out=out, in_=in_, func=func, scale=scale, **kw), rd, wr)

        def ts(eng, out, in0, s1, s2=None, op0=ALU.mult, op1=None, accum=None):
            rd = [in0] + [v for v in (s1, s2) if isap(v)]
            wr = [out] + ([accum] if accum is not None else [])
            kw = {}
            if op1 is not None:
                kw["op1"] = op1
            if accum is not None:
                kw["accum_out"] = accum
            return P.add(eng, lambda e: e.tensor_scalar(out=out, in0=in0, scalar1=s1, scalar2=s2, op0=op0, **kw), rd, wr)

        def tt(eng, out, in0, in1, op):
            return P.add(eng, lambda e: e.tensor_tensor(out=out, in0=in0, in1=in1, op=op), [in0, in1], [out])

        def stt(out, in0, scalar, in1, op0, op1):
            rd = [in0, in1] + ([scalar] if isap(scalar) else [])
            return P.add("dve", lambda e: e.scalar_tensor_tensor(out=out, in0=in0, scalar=scalar, in1=in1, op0=op0, op1=op1), rd, [out])

        def cp(eng, out, in_):
            if eng == "act":
                eng = "dve"
            return P.add(eng, lambda e: e.tensor_copy(out=out, in_=in_), [in_], [out])

        def evac(out, in_):
            st_flip[0] ^= 1
            return cp("act" if st_flip[0] else "dve", out, in_)

        def memset(eng, ap, val):
            return P.add(eng, lambda e: e.memset(ap, val), [], [ap])

        def red(out, in_, op):
            return P.add("dve", lambda e: e.tensor_reduce(out=out, in_=in_, axis=AX.X, op=op), [in_], [out])

        def recip(out, in_):
            return P.add("dve", lambda e: e.reciprocal(out=out, in_=in_), [in_], [out])

        def wload(dst, src, eng="pool"):
            return P.dma(eng, dst, src)

        cst = A.alloc([NCONST], F32)
        P.dma("sp", cst, D["consts"])
        cbf = A.alloc([NCBF], BF16)
        wload(cbf, D["cbf"])
        ident = cbf[:, B_ID:B_ID + 128]
        T1 = cbf[:, B_T1:B_T1 + 128]
        T2 = cbf[:, B_T2:B_T2 + 128]
        ones = cbf[:, B_ONE:B_ONE + 128]
        negones = cbf[:, B_NEG1:B_NEG1 + 128]
        negm8 = cbf[:, B_NM8:B_NM8 + 1024]
        ident32 = cst[:, C_ID32:C_ID32 + 128]
        neghalf = A.alloc([1], F32)
        memset("pool", neghalf, -0.5)

        def sumsq(junk_, in_, acc_):
            tt("dve", junk_, in_, in_, ALU.mult)
            return red(acc_, junk_, ALU.add)

        def rstd_pool(out, ssq, n):
            ts("pool", out, ssq, 1.0 / n, EPS, op0=ALU.mult, op1=ALU.add)
            tt("pool", out, out, neghalf, ALU.pow)

        PS = A.psum

        def norm_to_T(xt, gcol, dstT, i, scr):
            junk, ss, rs, xs = scr
            sumsq(junk, xt, ss)
            rstd_pool(rs, ss, DM)
            stt(xs, xt, rs[:, 0:1], cst[:, gcol:gcol + 1024], ALU.mult, ALU.mult)
            pt_ = PS((i % 2) * 512, [8, 128], BF16)
            for kc in range(8):
                tr(pt_[:, kc, :], xs[:, kc * 128:(kc + 1) * 128], ident)
            cp("act", dstT[:, 0:4, i * 128:(i + 1) * 128], pt_[:, 0:4, :])
            cp("dve", dstT[:, 4:8, i * 128:(i + 1) * 128], pt_[:, 4:8, :])

        m_h = A.mark()
        hT = A.alloc([8, S], BF16)
        m_p0 = A.mark()
        xts = [A.alloc([DM], F32) for _ in range(2)]
        scr = (A.alloc([DM], BF16), A.alloc([1], F32), A.alloc([1], F32), A.alloc([DM], BF16))
        import os
        for i in range(int(os.environ.get("P0_TILES", NT))):
            xt = xts[i % 2]
            P.dma("sp", xt, xd[i * 128:(i + 1) * 128, :])
            norm_to_T(xt, C_GMIXB, hT, i, scr)
        A.release(m_p0)

        dumps = []

        def dump(name, src, n):
            if name in dbg:
                m = A.mark()
                t = A.alloc([2048], F32)
                import os
                for c0 in range(0, min(n, 2048 * int(os.environ.get('DUMPN', '99'))), 2048):
                    cp("dve", t, src[:, c0:c0 + 2048])
                    dumps.append(P.dma("sp", dbg[name][:, c0:c0 + 2048], t))
                A.release(m)
            if stop == name:
                P.wait_for("sp", dumps)
                print("ops:", P.nops, {e: len(P.ops[e]) for e in ENGS}, "sbuf peak", A.peak)
                P.emit()
                return True
            return False

        if dump("hT", hT.rearrange("p a b -> p (a b)"), 8 * S):
            return nc

        ynT = A.alloc([16, S], BF16, top=True)
        m_ssm = A.mark()
        wdt = A.alloc([8, 32], BF16)
        wload(wdt, D["wdt"])
        aneg = A.alloc([32], F32)
        act(aneg, cst[:, C_ALOG:C_ALOG + 32], AF.Exp)
        ts("dve", aneg, aneg, -1.0, None, op0=ALU.mult)
        dt_all = A.alloc([NT, 32], F32)
        adt_bf = A.alloc([NT, 32], BF16)
        eall = A.alloc([NT, 96], F32)
        t32 = A.alloc([32], F32)
        for i in range(NT):
            ps = PS(3584 + (i % 2) * 128, [32])
            for kc in range(8):
                mm(ps, hT[:, kc, i * 128:(i + 1) * 128], wdt[:, kc, :], start=(kc == 0), stop=(kc == 7))
            tt("dve", t32, ps, cst[:, C_DTB:C_DTB + 32], ALU.add)
            act(t32, t32, AF.Exp)
            act(dt_all[:, i, :], t32, AF.Ln, bias=1.0)
            tt("dve", adt_bf[:, i, :], dt_all[:, i, :], aneg, ALU.mult)
        for i in range(NT):
            ps = PS(3072 + (i % 2) * 128, [96])
            mm(ps[:, 0:32], T1, adt_bf[:, i, :])
            mm(ps[:, 32:64], T2, adt_bf[:, i, :])
            mm(ps[:, 64:96], ones, adt_bf[:, i, :])
            act(eall[:, i, :], ps, AF.Exp)

        wg = A.alloc([8, 1280], BF16)
        pre = A.alloc([6, 515], F32)
        acc = [A.alloc([512], F32) for _ in range(2)]
        xbcT = A.alloc([6, 512], BF16)
        xdt_tok = A.alloc([512], BF16)
        xsD_tok = A.alloc([512], BF16)
        xdec = A.alloc([512], BF16)
        B_tok = A.alloc([128], BF16)
        Dm = A.alloc([8, 128], BF16)
        Em = A.alloc([8, 128], BF16)
        cbT = A.alloc([128], BF16)
        MT = A.alloc([8, 128], BF16)
        state = A.alloc([512], F32)
        state_bf = A.alloc([512], BF16)
        yo = A.alloc([512], F32)
        ysb = A.alloc([512], F32)
        sz = A.alloc([512], F32)
        un = A.alloc([512], BF16)
        junk5 = A.alloc([512], BF16)
        ssq = A.alloc([1], F32)
        rsd = A.alloc([1], F32)

        def b3(ap2):
            return ap2.unsqueeze(2).to_broadcast([128, 8, 64])

        def v3(ap):
            return ap.rearrange("p (h d) -> p h d", h=8)

        for g in range(4):
            for kc in range(8):
                wload(wg[:, kc, :], D["wssm"][g, :, kc, :])
            for sb in range(4):
                tsb = slice(sb * 512, (sb + 1) * 512)
                for cc in range(6):
                    ps = PS((cc % 2) * 512, [512])
                    for kc in range(8):
                        mm(ps, wg[:, kc, 512 + cc * 128:512 + (cc + 1) * 128], hT[:, kc, tsb], start=(kc == 0), stop=(kc == 7))
                    if sb == 0:
                        memset("pool", pre[:, cc, 0:3], 0.0)
                    else:
                        cp("pool", pre[:, cc, 0:3], pre[:, cc, 512:515])
                    cp("act", pre[:, cc, 3:515], ps)
                    gc = g * 4 + cc if cc < 4 else (16 + g if cc == 4 else 20 + g)
                    a = acc[cc % 2]
                    w0 = C_CWS + gc * 4
                    ts("dve", a, pre[:, cc, 0:512], cst[:, w0:w0 + 1], None, op0=ALU.mult)
                    for j in range(1, 4):
                        stt(a, pre[:, cc, j:j + 512], cst[:, w0 + j:w0 + j + 1], a, ALU.mult, ALU.add)
                    act(xbcT[:, cc, :], a, AF.Silu, bias=cst[:, C_CBS + gc:C_CBS + gc + 1])
                for j in range(4):
                    i = sb * 4 + j
                    tl = slice(j * 128, (j + 1) * 128)
                    ti = slice(i * 128, (i + 1) * 128)
                    last = (i == NT - 1)
                    pxs = PS(3584, [4, 128], BF16)
                    for cc in range(4):
                        tr(pxs[:, cc, :], xbcT[:, cc, tl], ident)
                    pB = PS(3584 + 256, [128], BF16)
                    tr(pB, xbcT[:, 4, tl], ident)
                    pxs3 = pxs.rearrange("p a (b d) -> p (a b) d", b=2)
                    tt("dve", v3(xdt_tok), pxs3, b3(dt_all[:, i, g * 8:(g + 1) * 8]), ALU.mult)
                    tt("dve", v3(xsD_tok), pxs3, b3(cst[:, C_DSK + g * 8:C_DSK + (g + 1) * 8]), ALU.mult)
                    if not last:
                        cp("act", B_tok, pB)
                        tt("pool", v3(xdec), v3(xdt_tok), b3(eall[:, i, 32 + g * 8:32 + (g + 1) * 8]), ALU.mult)
                    tt("dve", Dm, adt_bf[:, i, g * 8:(g + 1) * 8].unsqueeze(2).to_broadcast([128, 8, 128]),
                       T1.unsqueeze(1).to_broadcast([128, 8, 128]), ALU.mult)
                    seg = PS(1024, [8, 128])
                    for h in range(8):
                        mm(seg[:, h, :], ident, negm8[:, 0:128], start=True, stop=False)
                        mm(seg[:, h, :], ones, Dm[:, h, :], start=False, stop=False)
                        mm(seg[:, h, :], Dm[:, h, :], negones, start=False, stop=True)
                    act(Em, seg, AF.Exp)
                    pcb = PS(3584 + 320, [128])
                    mm(pcb, xbcT[:, 4, tl], xbcT[:, 5, tl])
                    cp("dve", cbT, pcb)
                    tt("dve", MT, Em, cbT.unsqueeze(1).to_broadcast([128, 8, 128]), ALU.mult)
                    py = PS(2048, [512])
                    for h in range(8):
                        hs = slice(h * 64, (h + 1) * 64)
                        mm(py[:, hs], ident, xsD_tok[:, hs], start=True, stop=False)
                        mm(py[:, hs], MT[:, h, :], xdt_tok[:, hs], start=False, stop=True)
                    if i > 0:
                        pyo = PS(2560, [512])
                        mm(pyo, xbcT[:, 5, tl], state_bf)
                        tt("dve", v3(yo), v3(pyo), b3(eall[:, i, g * 8:(g + 1) * 8]), ALU.mult)
                        tt("dve", ysb, py, yo, ALU.add)
                    else:
                        cp("dve", ysb, py)
                    if not last:
                        pn = PS(3072, [512])
                        mm(pn, B_tok, xdec)
                        if i == 0:
                            cp("dve", state, pn)
                        else:
                            tt("pool", v3(state), v3(state), b3(eall[:, i, 64 + g * 8:64 + (g + 1) * 8]), ALU.mult)
                            tt("dve", state, state, pn, ALU.add)
                        cp("act", state_bf, state)
                    pz = PS((j % 2) * 512, [512])
                    for kc in range(8):
                        mm(pz, hT[:, kc, ti], wg[:, kc, 0:512], start=(kc == 0), stop=(kc == 7))
                    act(sz, pz, AF.Silu)
                    tt("dve", ysb, ysb, sz, ALU.mult)
                    sumsq(junk5, ysb, ssq)
                    rstd_pool(rsd, ssq, 512)
                    ts("dve", un, ysb, rsd[:, 0:1], None, op0=ALU.mult)
                    pun = PS(3584, [4, 128], BF16)
                    for cc in range(4):
                        tr(pun[:, cc, :], un[:, cc * 128:(cc + 1) * 128], ident)
                    for cc in range(4):
                        gsc = cst[:, C_GSSM + g * 4 + cc:C_GSSM + g * 4 + cc + 1]
                        o = ynT[:, g * 4 + cc, ti]
                        ts("dve", o, pun[:, cc, :], gsc, None, op0=ALU.mult)
        A.release(m_ssm)
        if dump("ynT", ynT.rearrange("p a b -> p (a b)"), 16 * S):
            return nc

        mT = A.alloc([8, S], BF16)
        m_so = A.mark()
        wso = [A.alloc([16, 128], BF16) for _ in range(2)]
        wgs = [A.alloc([8, 128], BF16) for _ in range(2)]
        sg = [A.alloc([512], F32) for _ in range(2)]
        for n in range(8):
            wload(wso[n % 2], D["wso"][n])
            wload(wgs[n % 2], D["wgs"][n])
            for sb in range(4):
                tsb = slice(sb * 512, (sb + 1) * 512)
                pa = PS((sb % 2) * 512, [512])
                pb_ = PS(1024 + (sb % 2) * 512, [512])
                for kc in range(16):
                    mm(pa, wso[n % 2][:, kc, :], ynT[:, kc, tsb], start=(kc == 0), stop=(kc == 15))
                for kc in range(8):
                    mm(pb_, wgs[n % 2][:, kc, :], hT[:, kc, tsb], start=(kc == 0), stop=(kc == 7))
                act(sg[sb % 2], pb_, AF.Sigmoid)
                tt("dve", mT[:, n, tsb], pa, sg[sb % 2], ALU.mult)
        A.release(m_so)
        if dump("m1", mT.rearrange("p a b -> p (a b)"), 8 * S):
            return nc
        A.release_top()

        m_att = A.mark()
        btb = A.alloc([2, 16, 128], BF16)
        m_tmp = A.mark()
        bt32 = A.alloc([2, 16, 128], F32)
        P.dma("sp", bt32, D["biasT"])
        for dl in range(2):
            tt("dve", btb[:, dl], bt32[:, dl], cst[:, C_B31:C_B31 + 16].unsqueeze(2).to_broadcast([128, 16, 128]), ALU.subtract)
        A.release(m_tmp)
        wq = A.alloc([8, 1024], BF16)
        wqi = A.alloc([8, 512], BF16)
        wki2 = A.alloc([8, 128], BF16)
        wtok = A.alloc([8, 136], BF16)
        wuk = A.alloc([8, 128], BF16)
        wuvT = A.alloc([16, 64], BF16)
        for kc in range(8):
            wload(wq[:, kc, :], D["wq"][:, kc, :])
        wload(wqi, D["wqi"])
        wload(wki2, D["wki2"])
        wload(wtok, D["wtok"])
        wload(wuk, D["wuk"])
        wload(wuvT, D["wuvT"])
        ckv_tok = A.alloc([NT, 129], BF16)
        ckvT = A.alloc([S], BF16)
        kiT2 = A.alloc([S], BF16)
        widx_all = A.alloc([NT, 8], F32)
        qT_sb = A.alloc([8, 256], BF16)
        qlatT = A.alloc([16, 256], BF16)
        qiT = A.alloc([4, 256], BF16)
        score = A.alloc([S], F32)
        rl0 = A.alloc([512], F32)
        rl = [rl0, rl0]
        maskq = A.alloc([S], BF16)
        maskT = A.alloc([NT, 128], BF16)
        tmpb = A.alloc([8, 128], F32)
        Pm = [A.alloc([8, 128], BF16) for _ in range(2)]
        PT0 = A.alloc([8, 128], BF16)
        PT = [PT0, PT0]
        rec = A.alloc([8], F32)
        att_lat = A.alloc([8, 128], BF16)
        att_latT = A.alloc([16, 128], BF16)
        attT_sb = A.alloc([8, 256], BF16)
        wao = [A.alloc([8, 128], BF16) for _ in range(2)]
        wga = [A.alloc([8, 128], BF16) for _ in range(2)]
        sga = A.alloc([256], F32)
        tmpa = A.alloc([256], F32)
        sm = {k: A.alloc([1], F32) for k in ("lo", "hi", "rng", "th", "cand", "cnt", "gs", "ssq", "rs")}
        steps = A.alloc([NIT], F32)


        memset("pool", ckv_tok[:, :, 128:129], 1.0)
        for i in range(NT):
            ti = slice(i * 128, (i + 1) * 128)
            ps = PS((i % 2) * 512, [136])
            for kc in range(8):
                mm(ps, hT[:, kc, ti], wtok[:, kc, :], start=(kc == 0), stop=(kc == 7))
            cp("dve", tmpa[:, 0:128], ps[:, 0:128])
            sumsq(maskq[:, 0:128], tmpa[:, 0:128], sm["ssq"])
            rstd_pool(sm["rs"], sm["ssq"], 128)
            stt(ckv_tok[:, i, 0:128], tmpa[:, 0:128], sm["rs"][:, 0:1], cst[:, C_KVG:C_KVG + 128], ALU.mult, ALU.mult)
            cp("dve", widx_all[:, i, :], ps[:, 128:136])
            pt_ = PS(3584 + (i % 2) * 64, [128], BF16)
            tr(pt_, ckv_tok[:, i, 0:128], ident)
            cp("act", ckvT[:, ti], pt_)
        for sb in range(4):
            tsb = slice(sb * 512, (sb + 1) * 512)
            ps = PS(1024 + (sb % 2) * 512, [512])
            for kc in range(8):
                mm(ps, wki2[:, kc, :], hT[:, kc, tsb], start=(kc == 0), stop=(kc == 7))
            evac(kiT2[:, tsb], ps)

        slot = [0]

        def pslot(n):
            slot[0] = (slot[0] + 1) % 4
            return PS(slot[0] * 512, [n])

        for sbk in range(8):
            T0 = sbk * 256
            tq2 = slice(T0, T0 + 256)
            for cc in range(8):
                ps = pslot(256)
                for kc in range(8):
                    mm(ps, wq[:, kc, cc * 128:(cc + 1) * 128], hT[:, kc, tq2], start=(kc == 0), stop=(kc == 7))
                evac(qT_sb[:, cc, :], ps)
            for h in range(16):
                hb = (h % 2) * 64
                ps = pslot(256)
                mm(ps, wuk[hb:hb + 64, h // 2, :], qT_sb[hb:hb + 64, h // 2, :])
                ts("dve", qlatT[:, h, :], ps, 0.125, None, op0=ALU.mult)
            for hc in range(4):
                ps = pslot(256)
                for kc in range(8):
                    mm(ps, wqi[:, kc, hc * 128:(hc + 1) * 128], hT[:, kc, tq2], start=(kc == 0), stop=(kc == 7))
                evac(qiT[:, hc, :], ps)
            for j in range(2):
                i = sbk * 2 + j
                tq = slice(j * 128, (j + 1) * 128)
                Lk = (i + 1) * 128
                if i >= 2:
                    for hi_ in range(8):
                        hb = (hi_ % 2) * 64
                        for s0 in range(0, Lk, 512):
                            n = min(512, Lk - s0)
                            ps = pslot(n)
                            mm(ps, qiT[hb:hb + 64, hi_ // 2, tq], kiT2[hb:hb + 64, s0:s0 + n])
                            r_ = rl[(s0 // 512) % 2][:, 0:n]
                            act(r_, ps, AF.Relu)
                            wcol = widx_all[:, i, hi_:hi_ + 1]
                            if hi_ == 0:
                                ts("dve", score[:, s0:s0 + n], r_, wcol, None, op0=ALU.mult)
                            else:
                                stt(score[:, s0:s0 + n], r_, wcol, score[:, s0:s0 + n], ALU.mult, ALU.add)
                    tt("dve", score[:, i * 128:Lk], score[:, i * 128:Lk], cst[:, C_NTRI:C_NTRI + 128], ALU.add)
                    red(sm["lo"], score[:, 0:i * 128], ALU.min)
                    red(sm["hi"], score[:, 0:Lk], ALU.max)
                    tt("dve", sm["rng"], sm["hi"], sm["lo"], ALU.subtract)
                    ts("dve", steps, cst[:, C_PW:C_PW + NIT], sm["rng"][:, 0:1], None, op0=ALU.mult)
                    cp("dve", sm["th"], sm["lo"])
                    for n_ in range(NIT):
                        tt("dve", sm["cand"], sm["th"], steps[:, n_:n_ + 1], ALU.add)
                        ts("dve", maskq[:, 0:Lk], score[:, 0:Lk], sm["cand"][:, 0:1], None, op0=ALU.is_ge, op1=ALU.add, accum=sm["cnt"])
                        ts("dve", sm["gs"], sm["cnt"], 255.5, steps[:, n_:n_ + 1], op0=ALU.is_ge, op1=ALU.mult)
                        tt("dve", sm["th"], sm["th"], sm["gs"], ALU.add)
                    ts("dve", maskq[:, 0:Lk], score[:, 0:Lk], sm["th"][:, 0:1], None, op0=ALU.is_ge)
                    for j0 in range(0, i + 1, 4):
                        nj = min(4, i + 1 - j0)
                        pm_ = PS(3584, [4, 128], BF16)
                        for jj in range(nj):
                            tr(pm_[:, jj, :], maskq[:, (j0 + jj) * 128:(j0 + jj + 1) * 128], ident)
                        evac(maskT[:, j0:j0 + nj, :], pm_[:, 0:nj, :])
                for hg in range(2):
                    for jc in range(i + 1):
                        par = jc % 2
                        lg = PS(par * 1024, [8, 128])
                        for a in range(2):
                            mm(lg[:, a * 4:(a + 1) * 4, :], ckvT[:, jc * 128:(jc + 1) * 128],
                               qlatT[:, hg * 8 + a * 4:hg * 8 + (a + 1) * 4, tq])
                        dl = i - jc
                        if dl <= 1:
                            tt("dve", tmpb, lg, btb[:, dl, hg * 8:(hg + 1) * 8, :], ALU.add)
                            act(Pm[par], tmpb, AF.Exp)
                        else:
                            act(Pm[par], lg, AF.Exp)
                        if i >= 2:
                            mk = maskT[:, jc, :]
                        elif jc == i:
                            mk = T1
                        else:
                            mk = None
                        if mk is not None:
                            tt("pool", PT[par], Pm[par], mk.unsqueeze(1).to_broadcast([128, 8, 128]), ALU.mult)
                            pt_use = PT[par]
                        else:
                            pt_use = Pm[par]
                        for h8 in range(8):
                            o_ = PS((4 + h8 // 3) * 512 + (h8 % 3) * 129, [129])
                            mm(o_, pt_use[:, h8, :], ckv_tok[:, jc, :], start=(jc == 0 and h8 % 3 == 0), stop=(jc == i),
                               skip_group_check=True)
                    for bi, (h0, h1) in enumerate(((0, 3), (3, 6), (6, 8))):
                        ob = PS((4 + bi) * 512, [h1 - h0, 129])
                        recip(rec[:, h0:h1], ob[:, :, 128])
                        tt("dve", att_lat[:, h0:h1, :], ob[:, :, 0:128],
                           rec[:, h0:h1].unsqueeze(2).to_broadcast([128, h1 - h0, 128]), ALU.mult)
                    pl = PS(3584, [8, 128], BF16)
                    for h8 in range(8):
                        tr(pl[:, h8, :], att_lat[:, h8, :], ident)
                    evac(att_latT[:, hg * 8:hg * 8 + 4, :], pl[:, 0:4, :])
                    evac(att_latT[:, hg * 8 + 4:hg * 8 + 8, :], pl[:, 4:8, :])
                aps = PS(0, [8, 128])
                for h in range(16):
                    hb = (h % 2) * 64
                    mm(aps[hb:hb + 64, h // 2, :], wuvT[:, h, :], att_latT[:, h, :])
                evac(attT_sb[:, :, tq], aps)
            for n in range(8):
                wload(wao[n % 2], D["wao"][n])
                wload(wga[n % 2], D["wga"][n])
                pa = PS(1024 + (n % 2) * 256, [256])
                pb_ = PS(1536 + (n % 2) * 256, [256])
                for kc in range(8):
                    mm(pa, wao[n % 2][:, kc, :], attT_sb[:, kc, :], start=(kc == 0), stop=(kc == 7))
                for kc in range(8):
                    mm(pb_, wga[n % 2][:, kc, :], hT[:, kc, tq2], start=(kc == 0), stop=(kc == 7))
                act(sga, pb_, AF.Sigmoid)
                tt("dve", tmpa, pa, sga, ALU.mult)
                tt("dve", mT[:, n, tq2], mT[:, n, tq2], tmpa, ALU.add)
        A.release(m_att)
        if dump("m2", mT.rearrange("p a b -> p (a b)"), 8 * S):
            return nc

        h2T = hT
        m_c1 = A.mark()
        wout = A.alloc([8, 1024], BF16)
        for kc in range(8):
            wload(wout[:, kc, :], D["wout"][:, kc, :])
        xts = [A.alloc([DM], F32) for _ in range(2)]
        x1s = [A.alloc([DM], F32) for _ in range(2)]
        scr = (A.alloc([DM], BF16), A.alloc([1], F32), A.alloc([1], F32), A.alloc([DM], BF16))
        for i in range(NT):
            ti = slice(i * 128, (i + 1) * 128)
            xt = xts[i % 2]
            x1 = x1s[i % 2]
            P.dma("sp", xt, xd[ti, :])
            ps = PS(2048 + (i % 2) * 1024, [1024])
            for hf in range(2):
                for kc in range(8):
                    mm(ps[:, hf * 512:(hf + 1) * 512], mT[:, kc, ti], wout[:, kc, hf * 512:(hf + 1) * 512],
                       start=(kc == 0), stop=(kc == 7))
            tt("dve", x1, xt, ps, ALU.add)
            P.dma("sp", x1d[ti, :], x1)
            norm_to_T(x1, C_GFFNB, h2T, i, scr)
        A.release(m_c1)

        prodT = A.alloc([22, S], BF16, top=True)
        m_c2 = A.mark()
        wup = [A.alloc([8, 256], BF16) for _ in range(2)]
        preG = A.alloc([1026], F32)
        preV = A.alloc([1026], F32)
        accG = A.alloc([1024], F32)
        accV = A.alloc([1024], F32)
        sgG = A.alloc([1024], F32)
        for c in range(22):
            wu = wup[c % 2]
            wload(wu, D["wup"][c])
            for hf in range(2):
                pg = PS(hf * 2048, [1024])
                pv = PS(hf * 2048 + 1024, [1024])
                for q in range(2):
                    tcs = slice(hf * 1024 + q * 512, hf * 1024 + (q + 1) * 512)
                    for kc in range(8):
                        mm(pg[:, q * 512:(q + 1) * 512], wu[:, kc, 0:128], h2T[:, kc, tcs], start=(kc == 0), stop=(kc == 7))
                    for kc in range(8):
                        mm(pv[:, q * 512:(q + 1) * 512], wu[:, kc, 128:256], h2T[:, kc, tcs], start=(kc == 0), stop=(kc == 7))
                for pre_, pp_ in ((preG, pg), (preV, pv)):
                    if hf == 0:
                        memset("pool", pre_[:, 0:2], 0.0)
                    else:
                        cp("pool", pre_[:, 0:2], pre_[:, 1024:1026])
                    cp("act", pre_[:, 2:1026], pp_)
                for pre_, acc_, cch in ((preG, accG, c), (preV, accV, 22 + c)):
                    w0 = C_CWF + cch * 3
                    ts("dve", acc_, pre_[:, 0:1024], cst[:, w0:w0 + 1], None, op0=ALU.mult)
                    for j in range(1, 3):
                        stt(acc_, pre_[:, j:j + 1024], cst[:, w0 + j:w0 + j + 1], acc_, ALU.mult, ALU.add)
                act(sgG, accG, AF.Silu, bias=cst[:, C_CBF + c:C_CBF + c + 1])
                stt(prodT[:, c, hf * 1024:(hf + 1) * 1024], accV, cst[:, C_CBF + 22 + c:C_CBF + 22 + c + 1], sgG, ALU.add, ALU.mult)
        A.release(m_c2)

        A.release(m_h)
        wdn = A.alloc([22, 1024], BF16)
        for c in range(22):
            wload(wdn[:, c, :], D["wdn"][:, c, :])
        x1s = [A.alloc([DM], F32) for _ in range(2)]
        x2 = A.alloc([DM], F32)
        ots = [A.alloc([DM], F32) for _ in range(2)]
        junk = A.alloc([DM], BF16)
        ssf = A.alloc([1], F32)
        rsf = A.alloc([1], F32)
        outs = []
        for i in range(NT):
            ti = slice(i * 128, (i + 1) * 128)
            x1 = x1s[i % 2]
            P.dma("sp", x1, x1d[ti, :])
            ps = PS((i % 2) * 1024, [1024])
            for hf in range(2):
                for c in range(22):
                    mm(ps[:, hf * 512:(hf + 1) * 512], prodT[:, c, ti], wdn[:, c, hf * 512:(hf + 1) * 512],
                       start=(c == 0), stop=(c == 21))
            tt("dve", x2, x1, ps, ALU.add)
            sumsq(junk, x2, ssf)
            rstd_pool(rsf, ssf, DM)
            stt(ots[i % 2], x2, rsf[:, 0:1], cst[:, C_GFIN:C_GFIN + 1024], ALU.mult, ALU.mult)
            outs.append(P.dma("sp", outd[ti, :], ots[i % 2]))
        P.wait_for("sp", outs)
        print("ops:", P.nops, {e: len(P.ops[e]) for e in ENGS}, "sbuf peak", A.peak)
        P.emit()
    return nc


def _run(inputs, ncores=8, debug=(), stop=None):
    shared = _prep_shared(inputs)
    x = np.asarray(inputs["x"], np.float32)
    nc = build(debug, stop)
    in_maps = [dict(shared, x=np.ascontiguousarray(x[b])) for b in range(ncores)]
    res = run_bass_kernel_spmd(nc, in_maps, core_ids=list(range(ncores)))
    return res


def kernel(**inputs):
    res = _run(inputs, 8)
    return np.stack([np.asarray(res.results[b]["out"]) for b in range(8)]).astype(np.float32)
```

```python
import numpy as np
import concourse.bass as bass
import concourse.mybir as mybir

F32 = mybir.dt.float32
BF16 = mybir.dt.bfloat16
AF = mybir.ActivationFunctionType
ALU = mybir.AluOpType
AX = mybir.AxisListType

SCHED_LOG = None
ENGS = ("pe", "dve", "act", "pool", "sp")
_ESZ = {F32: 4, BF16: 2}


def _esize(dt):
    return _ESZ[dt]


class _Op:
    __slots__ = ("eng", "fn", "deps", "needs_inc", "sig", "is_dma", "dsem", "dcount", "prev_dma", "idx")

    def __init__(self, eng, fn, is_dma=False):
        self.eng = eng
        self.fn = fn
        self.deps = []
        self.needs_inc = False
        self.sig = 0
        self.is_dma = is_dma
        self.dsem = None
        self.dcount = 0
        self.prev_dma = None


class Prog:
    NDMA = {"sp": 10, "pool": 6, "act": 4}

    def __init__(self, nc):
        self.nc = nc
        self.ops = {e: [] for e in ENGS}
        self.recs = {}
        self.dma_rr = {e: 0 for e in self.NDMA}
        self.dma_last = {}
        self.dma_cnt = {}
        self.nops = 0

    def region(self, ap):
        t = ap.tensor
        name = t.name
        es = _esize(ap.dtype)
        dims = ap.ap
        if name in ("arena", "psum"):
            pstride = dims[0][0]
            npart = dims[0][1]
            p0 = ap.offset // pstride
            rem = ap.offset % pstride
            ext = 0
            for st, cnt in dims[1:]:
                ext += (cnt - 1) * abs(st)
            if name == "psum":
                lo = (rem * es) // 2048 * 2048
                hi = ((rem + ext + 1) * es + 2047) // 2048 * 2048
                return ("ps", 0, 128, lo, hi)
            return ("sb", p0, p0 + npart, rem * es, (rem + ext + 1) * es)
        ext = 0
        for st, cnt in dims:
            ext += (cnt - 1) * abs(st)
        return ("d:" + name, 0, 1, ap.offset * es, (ap.offset + ext + 1) * es)

    def _track(self, op, reads, writes):
        deps = set()
        for ap in reads:
            sp, p0, p1, b0, b1 = self.region(ap)
            lst = self.recs.setdefault(sp, [])
            for r in lst:
                if r[5] == "w" and r[0] < p1 and p0 < r[1] and r[2] < b1 and b0 < r[3]:
                    deps.add((r[4], "raw"))
        for ap in writes:
            sp, p0, p1, b0, b1 = self.region(ap)
            lst = self.recs.setdefault(sp, [])
            for r in lst:
                if r[0] < p1 and p0 < r[1] and r[2] < b1 and b0 < r[3]:
                    deps.add((r[4], "raw" if False else ("waw" if r[5] == "w" else "war")))
        for ap in reads:
            sp, p0, p1, b0, b1 = self.region(ap)
            lst = self.recs[sp]
            found = False
            for r in lst:
                if r[5] == "r" and r[4].eng == op.eng and r[0] == p0 and r[1] == p1 and r[2] == b0 and r[3] == b1:
                    r[4] = op
                    found = True
                    break
            if not found:
                lst.append([p0, p1, b0, b1, op, "r"])
        for ap in writes:
            sp, p0, p1, b0, b1 = self.region(ap)
            lst = self.recs[sp]
            lst[:] = [r for r in lst if not (r[0] >= p0 and r[1] <= p1 and r[2] >= b0 and r[3] <= b1)]
            lst.append([p0, p1, b0, b1, op, "w"])
        best = {}
        for prod, kind in deps:
            if prod is op:
                continue
            if prod.eng == op.eng and not prod.is_dma and not op.is_dma:
                if op.eng == "pe":
                    continue
            key = prod.dsem if prod.is_dma else prod.eng
            cur = best.get(key)
            if cur is None or prod.idx > cur.idx:
                best[key] = prod
        for prod in best.values():
            op.deps.append(prod)
            prod.needs_inc = True

    def add(self, eng, fn, reads=(), writes=()):
        op = _Op(eng, fn)
        op.idx = len(self.ops[eng])
        self.ops[eng].append(op)
        self._track(op, reads, writes)
        self.nops += 1
        return op

    def dma(self, eng, out, in_, **kw):
        op = _Op(eng, None, is_dma=True)
        j = self.dma_rr[eng]
        self.dma_rr[eng] = (j + 1) % self.NDMA[eng]
        key = (eng, j)
        op.dsem = key
        op.prev_dma = self.dma_last.get(key)
        op.dcount = self.dma_cnt.get(key, 0) + 1
        self.dma_cnt[key] = op.dcount
        self.dma_last[key] = op
        op.fn = lambda e: e.dma_start(out=out, in_=in_, **kw)
        op.idx = len(self.ops[eng])
        self.ops[eng].append(op)
        self._track(op, [in_], [out])
        self.nops += 1
        return op

    def wait_for(self, eng, prods):
        op = _Op(eng, None)
        op.idx = len(self.ops[eng])
        for p in prods:
            op.deps.append(p)
            p.needs_inc = True
        self.ops[eng].append(op)
        return op

    def emit(self):
        nc = self.nc
        for e in ENGS:
            c = 0
            for op in self.ops[e]:
                if op.is_dma:
                    continue
                if op.needs_inc:
                    c += 1
                    op.sig = c
        import contextlib
        with contextlib.ExitStack() as st:
            esem = {e: st.enter_context(nc.semaphore("s_" + e)) for e in ENGS}
            dsem = {}
            for e, n in self.NDMA.items():
                for j in range(n):
                    dsem[(e, j)] = st.enter_context(nc.semaphore("d_%s%d" % (e, j)))
            block = st.enter_context(nc.Block())
            engobj = {"pe": block.tensor, "dve": block.vector, "act": block.scalar, "pool": block.gpsimd, "sp": block.sync}

            def make(e):
                def body(eng):
                    waited = {}
                    for op in self.ops[e]:
                        need = []
                        for p in op.deps:
                            if p.is_dma:
                                need.append((("d",) + p.dsem, dsem[p.dsem], 16 * p.dcount))
                            else:
                                need.append((("e", p.eng), esem[p.eng], p.sig))
                        if op.is_dma and op.prev_dma is not None:
                            p = op.prev_dma
                            need.append((("d",) + p.dsem, dsem[p.dsem], 16 * p.dcount))
                        for key, sem, val in need:
                            if waited.get(key, 0) >= val:
                                continue
                            waited[key] = val
                            eng.wait_ge(sem, val)
                            if SCHED_LOG is not None:
                                SCHED_LOG.append("%s wait %s >= %d" % (e, key, val))
                        if SCHED_LOG is not None:
                            SCHED_LOG.append("%s op#%d %s inc=%s" % (e, op.idx, "dma" if op.is_dma else ("wait" if op.fn is None else "op"),
                                             (op.dsem if op.is_dma else (op.sig if op.needs_inc else None))))
                        if op.fn is None:
                            continue
                        ins = op.fn(eng)
                        if op.is_dma:
                            ins.then_inc(dsem[op.dsem], 16)
                        elif op.needs_inc:
                            ins.then_inc(esem[e], 1)
                return body

            for e in ENGS:
                engobj[e](make(e))


class Arena:
    def __init__(self, nc, st, kb=200):
        self.t = st.enter_context(nc.sbuf_tensor("arena", [128, kb * 256], F32))
        self.ps = st.enter_context(nc.psum_tensor("psum", [128, 4096], F32))
        self.top = 0
        self.cap = kb * 1024
        self.cap0 = kb * 1024
        self.peak = 0

    def alloc(self, shape, dtype, top=False):
        es = _esize(dtype)
        n = 1
        for s in shape:
            n *= s
        nb = (n * es + 63) // 64 * 64
        if top:
            self.cap -= nb
            off = self.cap
        else:
            off = self.top
            self.top += nb
        self.peak = max(self.peak, self.top)
        assert self.top <= self.cap, "SBUF arena overflow %d > %d" % (self.top, self.cap)
        v = self.t[:, off // 4:(off + nb) // 4].bitcast(dtype)[:, 0:n]
        return _reshape(v, shape)

    def mark(self):
        return self.top

    def release(self, m):
        self.top = m

    def release_top(self):
        self.cap = self.cap0

    def psum(self, off_f32, shape, dtype=F32):
        es = _esize(dtype)
        n = 1
        for s in shape:
            n *= s
        nw = (n * es + 3) // 4
        v = self.ps[:, off_f32:off_f32 + nw]
        if dtype != F32:
            v = v.bitcast(dtype)[:, 0:n]
        return _reshape(v, shape)


def _reshape(v, shape):
    if len(shape) == 1:
        return v
    names = "abcdefg"[:len(shape)]
    pat = "p (%s) -> p %s" % (" ".join(names), " ".join(names))
    kw = {names[k]: shape[k] for k in range(len(shape))}
    return v.rearrange(pat, **kw)

from concourse.bass_utils import run_bass_kernel_spmd
import contextlib

S = 2048
DM = 1024
NT = 16
EPS = 1e-6
OQ, OC, OQI, OKI, OWI, OZ, OXBC, ODT, OGA, OGS = 0, 1024, 1152, 1664, 1728, 1736, 3784, 6856, 6888, 7912
NIT = 24
C_GMIX, C_GFFN, C_GSSM, C_CWS, C_CBS, C_CWF, C_CBF = 0, 8, 16, 32, 128, 152, 284
C_DTB, C_ALOG, C_DSK, C_KVG, C_B31, C_GFIN, C_ID32, C_NTRI, C_PW = 328, 360, 392, 424, 552, 568, 1592, 1720, 1848
C_GMIXB = C_PW + NIT
C_GFFNB = C_GMIXB + 1024
NCONST = C_GFFNB + 1024
B_ID, B_T1, B_T2, B_ONE, B_NEG1, B_NM8 = 0, 128, 256, 384, 512, 640
NCBF = B_NM8 + 1024


def _t5_bucket(n):
    n = np.maximum(n, 0)
    max_exact = 16
    large = max_exact + (np.log(np.maximum(n, 1).astype(np.float32) / max_exact)
                         / np.float32(np.log(128 / max_exact)) * (32 - max_exact)).astype(np.int32)
    large = np.minimum(large, 31)
    return np.where(n < max_exact, n, large)


def _prep_shared(inp):
    f = np.float32
    w_in = np.asarray(inp["w_in"], f)[0]

    def pk(cols):
        a = w_in[:, cols]
        return np.ascontiguousarray(a.reshape(8, 128, a.shape[1]).transpose(1, 0, 2))

    def rows_pk(w, nk):
        return np.ascontiguousarray(w.reshape(nk, 128, w.shape[1]).transpose(1, 0, 2))

    d = {}
    d["wq"] = pk(np.arange(OQ, OQ + 1024))
    d["wqi"] = pk(np.arange(OQI, OQI + 512))
    d["wki2"] = pk(np.concatenate([np.arange(OKI, OKI + 64)] * 2))
    d["wtok"] = pk(np.concatenate([np.arange(OC, OC + 128), np.arange(OWI, OWI + 8)]))
    d["wdt"] = pk(np.arange(ODT, ODT + 32))
    d["wga"] = np.stack([pk(np.arange(OGA + n * 128, OGA + (n + 1) * 128)) for n in range(8)])
    d["wgs"] = np.stack([pk(np.arange(OGS + n * 128, OGS + (n + 1) * 128)) for n in range(8)])
    ws = []
    for g in range(4):
        cols = np.concatenate([np.arange(OZ + g * 512, OZ + (g + 1) * 512),
                               np.arange(OXBC + g * 512, OXBC + (g + 1) * 512),
                               np.arange(OXBC + 2048 + g * 128, OXBC + 2048 + (g + 1) * 128),
                               np.arange(OXBC + 2560 + g * 128, OXBC + 2560 + (g + 1) * 128)])
        ws.append(pk(cols))
    d["wssm"] = np.stack(ws)
    wso = np.asarray(inp["w_ssm_out"], f)[0]
    d["wso"] = np.stack([rows_pk(wso[:, n * 128:(n + 1) * 128], 16) for n in range(8)])
    wao = np.asarray(inp["w_att_out"], f)[0]
    d["wao"] = np.stack([rows_pk(wao[:, n * 128:(n + 1) * 128], 8) for n in range(8)])
    d["wout"] = rows_pk(np.asarray(inp["w_out"], f)[0], 8)
    wup = np.asarray(inp["w_ffn_up"], f)[0]
    d["wup"] = np.stack([rows_pk(np.concatenate([wup[:, c * 128:(c + 1) * 128],
                                                 wup[:, 2816 + c * 128:2816 + (c + 1) * 128]], axis=1), 8)
                         for c in range(22)])
    d["wdn"] = rows_pk(np.asarray(inp["w_ffn_down"], f)[0], 22)
    wuk = np.asarray(inp["w_uk"], f)[0]
    d["wuk"] = np.ascontiguousarray(wuk.reshape(8, 2, 64, 128).transpose(1, 2, 0, 3).reshape(128, 8, 128))
    wuv = np.asarray(inp["w_uv"], f)[0]
    d["wuvT"] = np.ascontiguousarray(wuv.transpose(2, 0, 1))

    cst = np.zeros((128, NCONST), f)

    def pp(v, nk):
        return np.asarray(v, f).reshape(nk, 128).T

    def bc(v):
        return np.broadcast_to(np.asarray(v, f).reshape(1, -1), (128, np.asarray(v).size))

    cst[:, C_GMIX:C_GMIX + 8] = pp(inp["norm_mix"][0], 8)
    cst[:, C_GFFN:C_GFFN + 8] = pp(inp["norm_ffn"][0], 8)
    cst[:, C_GSSM:C_GSSM + 16] = pp(inp["ssm_norm"][0], 16)
    cws = np.asarray(inp["conv_ssm_w"], f)[0]
    cst[:, C_CWS:C_CWS + 96] = cws.T.reshape(24, 128, 4).transpose(1, 0, 2).reshape(128, 96)
    cst[:, C_CBS:C_CBS + 24] = pp(inp["conv_ssm_b"][0], 24)
    cwf = np.asarray(inp["conv_ffn_w"], f)[0]
    cst[:, C_CWF:C_CWF + 132] = cwf.T.reshape(44, 128, 3).transpose(1, 0, 2).reshape(128, 132)
    cst[:, C_CBF:C_CBF + 44] = pp(inp["conv_ffn_b"][0], 44)
    cst[:, C_DTB:C_DTB + 32] = bc(inp["dt_bias"][0])
    cst[:, C_ALOG:C_ALOG + 32] = bc(inp["a_log"][0])
    cst[:, C_DSK:C_DSK + 32] = bc(inp["d_skip"][0])
    cst[:, C_KVG:C_KVG + 128] = bc(inp["kv_norm"][0])
    rb = np.asarray(inp["rel_bias"], f)
    cst[:, C_B31:C_B31 + 16] = bc(rb[31])
    cst[:, C_GFIN:C_GFIN + 1024] = bc(inp["norm_final"])
    r = np.arange(128)
    cst[:, C_ID32:C_ID32 + 128] = np.eye(128, dtype=f)
    cst[:, C_NTRI:C_NTRI + 128] = np.where(r[:, None] < r[None, :], f(-1e30), f(0))
    cst[:, C_PW:C_PW + NIT] = (0.5 ** np.arange(1, NIT + 1)).astype(f)[None, :]
    cst[:, C_GMIXB:C_GMIXB + 1024] = bc(inp["norm_mix"][0])
    cst[:, C_GFFNB:C_GFFNB + 1024] = bc(inp["norm_ffn"][0])
    d["consts"] = cst
    cb = np.zeros((128, NCBF), f)
    cb[:, B_ID:B_ID + 128] = np.eye(128)
    cb[:, B_T1:B_T1 + 128] = (r[:, None] <= r[None, :])
    cb[:, B_T2:B_T2 + 128] = (r[:, None] > r[None, :])
    cb[:, B_ONE:B_ONE + 128] = 1.0
    cb[:, B_NEG1:B_NEG1 + 128] = -1.0
    cb[:, B_NM8:B_NM8 + 1024] = np.tile(np.where(r[:, None] > r[None, :], f(-30000.0), f(0)), (1, 8))
    d["cbf"] = cb
    bt = np.zeros((128, 2, 16, 128), f)
    for dlt in range(2):
        dist = 128 * dlt + r[None, :] - r[:, None]
        bk = _t5_bucket(dist)
        g = rb[bk]
        bt[:, dlt] = g.transpose(0, 2, 1)
    d["biasT"] = bt
    return d


SHAPES = {"wq": [128, 8, 1024], "wqi": [128, 8, 512], "wki2": [128, 8, 128], "wtok": [128, 8, 136],
          "wdt": [128, 8, 32], "wga": [8, 128, 8, 128], "wgs": [8, 128, 8, 128], "wssm": [4, 128, 8, 1280],
          "wso": [8, 128, 16, 128], "wao": [8, 128, 8, 128], "wout": [128, 8, 1024], "wup": [22, 128, 8, 256],
          "wdn": [128, 22, 1024], "wuk": [128, 8, 128], "wuvT": [128, 16, 64], "consts": [128, NCONST],
          "cbf": [128, NCBF], "biasT": [128, 2, 16, 128]}


def build(debug=(), stop=None):
    nc = bass.Bass("TRN2", target_bir_lowering=False)
    D = {}
    for k, shp in SHAPES.items():
        D[k] = nc.dram_tensor(k, shp, F32, kind="ExternalInput").ap()
    xd = nc.dram_tensor("x", [S, DM], F32, kind="ExternalInput").ap()
    outd = nc.dram_tensor("out", [S, DM], F32, kind="ExternalOutput").ap()
    x1d = nc.dram_tensor("x1d", [S, DM], F32, kind="Internal").ap()
    dbg = {}
    if "hT" in debug:
        dbg["hT"] = nc.dram_tensor("dbg_hT", [128, 8 * S], F32, kind="ExternalOutput").ap()
    if "ynT" in debug:
        dbg["ynT"] = nc.dram_tensor("dbg_ynT", [128, 16 * S], F32, kind="ExternalOutput").ap()
    if "m1" in debug:
        dbg["m1"] = nc.dram_tensor("dbg_m1", [128, 8 * S], F32, kind="ExternalOutput").ap()
    if "m2" in debug:
        dbg["m2"] = nc.dram_tensor("dbg_m2", [128, 8 * S], F32, kind="ExternalOutput").ap()

    with contextlib.ExitStack() as st:
        A = Arena(nc, st, kb=206)
        P = Prog(nc)
        st_flip = [0]

        def isap(v):
            return not isinstance(v, (int, float)) and v is not None

        def mm(out, lhsT, rhs, start=True, stop=True, **kw):
            rd = [lhsT, rhs] + ([] if start else [out])
            return P.add("pe", lambda e: e.matmul(out, lhsT=lhsT, rhs=rhs, start=start, stop=stop, **kw), rd, [out])

        def tr(out, in_, ident):
            return P.add("pe", lambda e: e.transpose(out, in_, ident), [in_, ident], [out])

        def act(out, in_, func, bias=None, scale=1.0, accum=None):
            rd = [in_] + [v for v in (bias, scale) if isap(v)]
            wr = [out] + ([accum] if accum is not None else [])
            kw = {}
            if bias is not None:
                kw["bias"] = bias
            if accum is not None:
                kw["accum_out"] = accum
            return P.add("act", lambda e: e.activation(out=out, in_=in_, func=func, scale=scale, **kw), rd, wr)

        def ts(eng, out, in0, s1, s2=None, op0=ALU.mult, op1=None, accum=None):
            rd = [in0] + [v for v in (s1, s2) if isap(v)]
            wr = [out] + ([accum] if accum is not None else [])
            kw = {}
            if op1 is not None:
                kw["op1"] = op1
            if accum is not None:
                kw["accum_out"] = accum
            return P.add(eng, lambda e: e.tensor_scalar(out=out, in0=in0, scalar1=s1, scalar2=s2, op0=op0, **kw), rd, wr)

        def tt(eng, out, in0, in1, op):
            return P.add(eng, lambda e: e.tensor_tensor(out=out, in0=in0, in1=in1, op=op), [in0, in1], [out])

        def stt(out, in0, scalar, in1, op0, op1):
            rd = [in0, in1] + ([scalar] if isap(scalar) else [])
            return P.add("dve", lambda e: e.scalar_tensor_tensor(out=out, in0=in0, scalar=scalar, in1=in1, op0=op0, op1=op1), rd, [out])

        def cp(eng, out, in_):
            if eng == "act":
                eng = "dve"
            return P.add(eng, lambda e: e.tensor_copy(out=out, in_=in_), [in_], [out])

        def evac(out, in_):
            st_flip[0] ^= 1
            return cp("act" if st_flip[0] else "dve", out, in_)

        def memset(eng, ap, val):
            return P.add(eng, lambda e: e.memset(ap, val), [], [ap])

        def red(out, in_, op):
            return P.add("dve", lambda e: e.tensor_reduce(out=out, in_=in_, axis=AX.X, op=op), [in_], [out])

        def recip(out, in_):
            return P.add("dve", lambda e: e.reciprocal(out=out, in_=in_), [in_], [out])

        def wload(dst, src, eng="pool"):
            return P.dma(eng, dst, src)

        cst = A.alloc([NCONST], F32)
        P.dma("sp", cst, D["consts"])
        cbf = A.alloc([NCBF], BF16)
        wload(cbf, D["cbf"])
        ident = cbf[:, B_ID:B_ID + 128]
        T1 = cbf[:, B_T1:B_T1 + 128]
        T2 = cbf[:, B_T2:B_T2 + 128]
        ones = cbf[:, B_ONE:B_ONE + 128]
        negones = cbf[:, B_NEG1:B_NEG1 + 128]
        negm8 = cbf[:, B_NM8:B_NM8 + 1024]
        ident32 = cst[:, C_ID32:C_ID32 + 128]
        neghalf = A.alloc([1], F32)
        memset("pool", neghalf, -0.5)

        def sumsq(junk_, in_, acc_):
            tt("dve", junk_, in_, in_, ALU.mult)
            return red(acc_, junk_, ALU.add)

        def rstd_pool(out, ssq, n):
            ts("pool", out, ssq, 1.0 / n, EPS, op0=ALU.mult, op1=ALU.add)
            tt("pool", out, out, neghalf, ALU.pow)

        PS = A.psum

        def norm_to_T(xt, gcol, dstT, i, scr):
            junk, ss, rs, xs = scr
            sumsq(junk, xt, ss)
            rstd_pool(rs, ss, DM)
            stt(xs, xt, rs[:, 0:1], cst[:, gcol:gcol + 1024], ALU.mult, ALU.mult)
            pt_ = PS((i % 2) * 512, [8, 128], BF16)
            for kc in range(8):
                tr(pt_[:, kc, :], xs[:, kc * 128:(kc + 1) * 128], ident)
            cp("act", dstT[:, 0:4, i * 128:(i + 1) * 128], pt_[:, 0:4, :])
            cp("dve", dstT[:, 4:8, i * 128:(i + 1) * 128], pt_[:, 4:8, :])

        m_h = A.mark()
        hT = A.alloc([8, S], BF16)
        m_p0 = A.mark()
        xts = [A.alloc([DM], F32) for _ in range(2)]
        scr = (A.alloc([DM], BF16), A.alloc([1], F32), A.alloc([1], F32), A.alloc([DM], BF16))
        import os
        for i in range(int(os.environ.get("P0_TILES", NT))):
            xt = xts[i % 2]
            P.dma("sp", xt, xd[i * 128:(i + 1) * 128, :])
            norm_to_T(xt, C_GMIXB, hT, i, scr)
        A.release(m_p0)

        dumps = []

        def dump(name, src, n):
            if name in dbg:
                m = A.mark()
                t = A.alloc([2048], F32)
                import os
                for c0 in range(0, min(n, 2048 * int(os.environ.get('DUMPN', '99'))), 2048):
                    cp("dve", t, src[:, c0:c0 + 2048])
                    dumps.append(P.dma("sp", dbg[name][:, c0:c0 + 2048], t))
                A.release(m)
            if stop == name:
                P.wait_for("sp", dumps)
                print("ops:", P.nops, {e: len(P.ops[e]) for e in ENGS}, "sbuf peak", A.peak)
                P.emit()
                return True
            return False

        if dump("hT", hT.rearrange("p a b -> p (a b)"), 8 * S):
            return nc

        ynT = A.alloc([16, S], BF16, top=True)
        m_ssm = A.mark()
        wdt = A.alloc([8, 32], BF16)
        wload(wdt, D["wdt"])
        aneg = A.alloc([32], F32)
        act(aneg, cst[:, C_ALOG:C_ALOG + 32], AF.Exp)
        ts("dve", aneg, aneg, -1.0, None, op0=ALU.mult)
        dt_all = A.alloc([NT, 32], F32)
        adt_bf = A.alloc([NT, 32], BF16)
        eall = A.alloc([NT, 96], F32)
        t32 = A.alloc([32], F32)
        for i in range(NT):
            ps = PS(3584 + (i % 2) * 128, [32])
            for kc in range(8):
                mm(ps, hT[:, kc, i * 128:(i + 1) * 128], wdt[:, kc, :], start=(kc == 0), stop=(kc == 7))
            tt("dve", t32, ps, cst[:, C_DTB:C_DTB + 32], ALU.add)
            act(t32, t32, AF.Exp)
            act(dt_all[:, i, :], t32, AF.Ln, bias=1.0)
            tt("dve", adt_bf[:, i, :], dt_all[:, i, :], aneg, ALU.mult)
        for i in range(NT):
            ps = PS(3072 + (i % 2) * 128, [96])
            mm(ps[:, 0:32], T1, adt_bf[:, i, :])
            mm(ps[:, 32:64], T2, adt_bf[:, i, :])
            mm(ps[:, 64:96], ones, adt_bf[:, i, :])
            act(eall[:, i, :], ps, AF.Exp)

        wg = A.alloc([8, 1280], BF16)
        pre = A.alloc([6, 515], F32)
        acc = [A.alloc([512], F32) for _ in range(2)]
        xbcT = A.alloc([6, 512], BF16)
        xdt_tok = A.alloc([512], BF16)
        xsD_tok = A.alloc([512], BF16)
        xdec = A.alloc([512], BF16)
        B_tok = A.alloc([128], BF16)
        Dm = A.alloc([8, 128], BF16)
        Em = A.alloc([8, 128], BF16)
        cbT = A.alloc([128], BF16)
        MT = A.alloc([8, 128], BF16)
        state = A.alloc([512], F32)
        state_bf = A.alloc([512], BF16)
        yo = A.alloc([512], F32)
        ysb = A.alloc([512], F32)
        sz = A.alloc([512], F32)
        un = A.alloc([512], BF16)
        junk5 = A.alloc([512], BF16)
        ssq = A.alloc([1], F32)
        rsd = A.alloc([1], F32)

        def b3(ap2):
            return ap2.unsqueeze(2).to_broadcast([128, 8, 64])

        def v3(ap):
            return ap.rearrange("p (h d) -> p h d", h=8)

        for g in range(4):
            for kc in range(8):
                wload(wg[:, kc, :], D["wssm"][g, :, kc, :])
            for sb in range(4):
                tsb = slice(sb * 512, (sb + 1) * 512)
                for cc in range(6):
                    ps = PS((cc % 2) * 512, [512])
                    for kc in range(8):
                        mm(ps, wg[:, kc, 512 + cc * 128:512 + (cc + 1) * 128], hT[:, kc, tsb], start=(kc == 0), stop=(kc == 7))
                    if sb == 0:
                        memset("pool", pre[:, cc, 0:3], 0.0)
                    else:
                        cp("pool", pre[:, cc, 0:3], pre[:, cc, 512:515])
                    cp("act", pre[:, cc, 3:515], ps)
                    gc = g * 4 + cc if cc < 4 else (16 + g if cc == 4 else 20 + g)
                    a = acc[cc % 2]
                    w0 = C_CWS + gc * 4
                    ts("dve", a, pre[:, cc, 0:512], cst[:, w0:w0 + 1], None, op0=ALU.mult)
                    for j in range(1, 4):
                        stt(a, pre[:, cc, j:j + 512], cst[:, w0 + j:w0 + j + 1], a, ALU.mult, ALU.add)
                    act(xbcT[:, cc, :], a, AF.Silu, bias=cst[:, C_CBS + gc:C_CBS + gc + 1])
                for j in range(4):
                    i = sb * 4 + j
                    tl = slice(j * 128, (j + 1) * 128)
                    ti = slice(i * 128, (i + 1) * 128)
                    last = (i == NT - 1)
                    pxs = PS(3584, [4, 128], BF16)
                    for cc in range(4):
                        tr(pxs[:, cc, :], xbcT[:, cc, tl], ident)
                    pB = PS(3584 + 256, [128], BF16)
                    tr(pB, xbcT[:, 4, tl], ident)
                    pxs3 = pxs.rearrange("p a (b d) -> p (a b) d", b=2)
                    tt("dve", v3(xdt_tok), pxs3, b3(dt_all[:, i, g * 8:(g + 1) * 8]), ALU.mult)
                    tt("dve", v3(xsD_tok), pxs3, b3(cst[:, C_DSK + g * 8:C_DSK + (g + 1) * 8]), ALU.mult)
                    if not last:
                        cp("act", B_tok, pB)
                        tt("pool", v3(xdec), v3(xdt_tok), b3(eall[:, i, 32 + g * 8:32 + (g + 1) * 8]), ALU.mult)
                    tt("dve", Dm, adt_bf[:, i, g * 8:(g + 1) * 8].unsqueeze(2).to_broadcast([128, 8, 128]),
                       T1.unsqueeze(1).to_broadcast([128, 8, 128]), ALU.mult)
                    seg = PS(1024, [8, 128])
                    for h in range(8):
                        mm(seg[:, h, :], ident, negm8[:, 0:128], start=True, stop=False)
                        mm(seg[:, h, :], ones, Dm[:, h, :], start=False, stop=False)
                        mm(seg[:, h, :], Dm[:, h, :], negones, start=False, stop=True)
                    act(Em, seg, AF.Exp)
                    pcb = PS(3584 + 320, [128])
                    mm(pcb, xbcT[:, 4, tl], xbcT[:, 5, tl])
                    cp("dve", cbT, pcb)
                    tt("dve", MT, Em, cbT.unsqueeze(1).to_broadcast([128, 8, 128]), ALU.mult)
                    py = PS(2048, [512])
                    for h in range(8):
                        hs = slice(h * 64, (h + 1) * 64)
                        mm(py[:, hs], ident, xsD_tok[:, hs], start=True, stop=False)
                        mm(py[:, hs], MT[:, h, :], xdt_tok[:, hs], start=False, stop=True)
                    if i > 0:
                        pyo = PS(2560, [512])
                        mm(pyo, xbcT[:, 5, tl], state_bf)
                        tt("dve", v3(yo), v3(pyo), b3(eall[:, i, g * 8:(g + 1) * 8]), ALU.mult)
                        tt("dve", ysb, py, yo, ALU.add)
                    else:
                        cp("dve", ysb, py)
                    if not last:
                        pn = PS(3072, [512])
                        mm(pn, B_tok, xdec)
                        if i == 0:
                            cp("dve", state, pn)
                        else:
                            tt("pool", v3(state), v3(state), b3(eall[:, i, 64 + g * 8:64 + (g + 1) * 8]), ALU.mult)
                            tt("dve", state, state, pn, ALU.add)
                        cp("act", state_bf, state)
                    pz = PS((j % 2) * 512, [512])
                    for kc in range(8):
                        mm(pz, hT[:, kc, ti], wg[:, kc, 0:512], start=(kc == 0), stop=(kc == 7))
                    act(sz, pz, AF.Silu)
                    tt("dve", ysb, ysb, sz, ALU.mult)
                    sumsq(junk5, ysb, ssq)
                    rstd_pool(rsd, ssq, 512)
                    ts("dve", un, ysb, rsd[:, 0:1], None, op0=ALU.mult)
                    pun = PS(3584, [4, 128], BF16)
                    for cc in range(4):
                        tr(pun[:, cc, :], un[:, cc * 128:(cc + 1) * 128], ident)
                    for cc in range(4):
                        gsc = cst[:, C_GSSM + g * 4 + cc:C_GSSM + g * 4 + cc + 1]
                        o = ynT[:, g * 4 + cc, ti]
                        ts("dve", o, pun[:, cc, :], gsc, None, op0=ALU.mult)
        A.release(m_ssm)
        if dump("ynT", ynT.rearrange("p a b -> p (a b)"), 16 * S):
            return nc

        mT = A.alloc([8, S], BF16)
        m_so = A.mark()
        wso = [A.alloc([16, 128], BF16) for _ in range(2)]
        wgs = [A.alloc([8, 128], BF16) for _ in range(2)]
        sg = [A.alloc([512], F32) for _ in range(2)]
        for n in range(8):
            wload(wso[n % 2], D["wso"][n])
            wload(wgs[n % 2], D["wgs"][n])
            for sb in range(4):
                tsb = slice(sb * 512, (sb + 1) * 512)
                pa = PS((sb % 2) * 512, [512])
                pb_ = PS(1024 + (sb % 2) * 512, [512])
                for kc in range(16):
                    mm(pa, wso[n % 2][:, kc, :], ynT[:, kc, tsb], start=(kc == 0), stop=(kc == 15))
                for kc in range(8):
                    mm(pb_, wgs[n % 2][:, kc, :], hT[:, kc, tsb], start=(kc == 0), stop=(kc == 7))
                act(sg[sb % 2], pb_, AF.Sigmoid)
                tt("dve", mT[:, n, tsb], pa, sg[sb % 2], ALU.mult)
        A.release(m_so)
        if dump("m1", mT.rearrange("p a b -> p (a b)"), 8 * S):
            return nc
        A.release_top()

        m_att = A.mark()
        btb = A.alloc([2, 16, 128], BF16)
        m_tmp = A.mark()
        bt32 = A.alloc([2, 16, 128], F32)
        P.dma("sp", bt32, D["biasT"])
        for dl in range(2):
            tt("dve", btb[:, dl], bt32[:, dl], cst[:, C_B31:C_B31 + 16].unsqueeze(2).to_broadcast([128, 16, 128]), ALU.subtract)
        A.release(m_tmp)
        wq = A.alloc([8, 1024], BF16)
        wqi = A.alloc([8, 512], BF16)
        wki2 = A.alloc([8, 128], BF16)
        wtok = A.alloc([8, 136], BF16)
        wuk = A.alloc([8, 128], BF16)
        wuvT = A.alloc([16, 64], BF16)
        for kc in range(8):
            wload(wq[:, kc, :], D["wq"][:, kc, :])
        wload(wqi, D["wqi"])
        wload(wki2, D["wki2"])
        wload(wtok, D["wtok"])
        wload(wuk, D["wuk"])
        wload(wuvT, D["wuvT"])
        ckv_tok = A.alloc([NT, 129], BF16)
        ckvT = A.alloc([S], BF16)
        kiT2 = A.alloc([S], BF16)
        widx_all = A.alloc([NT, 8], F32)
        qT_sb = A.alloc([8, 256], BF16)
        qlatT = A.alloc([16, 256], BF16)
        qiT = A.alloc([4, 256], BF16)
        score = A.alloc([S], F32)
        rl0 = A.alloc([512], F32)
        rl = [rl0, rl0]
        maskq = A.alloc([S], BF16)
        maskT = A.alloc([NT, 128], BF16)
        tmpb = A.alloc([8, 128], F32)
        Pm = [A.alloc([8, 128], BF16) for _ in range(2)]
        PT = [A.alloc([8, 128], BF16) for _ in range(2)]
        rec = A.alloc([8], F32)
        att_lat = A.alloc([8, 128], BF16)
        att_latT = A.alloc([16, 128], BF16)
        attT_sb = A.alloc([8, 256], BF16)
        wao = [A.alloc([8, 128], BF16) for _ in range(2)]
        wga = [A.alloc([8, 128], BF16) for _ in range(2)]
        sga = A.alloc([256], F32)
        tmpa = A.alloc([256], F32)
        sm = {k: A.alloc([1], F32) for k in ("lo", "hi", "rng", "th", "cand", "cnt", "gs", "ssq", "rs")}
        steps = A.alloc([NIT], F32)


        memset("pool", ckv_tok[:, :, 128:129], 1.0)
        for i in range(NT):
            ti = slice(i * 128, (i + 1) * 128)
            ps = PS((i % 2) * 512, [136])
            for kc in range(8):
                mm(ps, hT[:, kc, ti], wtok[:, kc, :], start=(kc == 0), stop=(kc == 7))
            cp("dve", tmpa[:, 0:128], ps[:, 0:128])
            sumsq(maskq[:, 0:128], tmpa[:, 0:128], sm["ssq"])
            rstd_pool(sm["rs"], sm["ssq"], 128)
            stt(ckv_tok[:, i, 0:128], tmpa[:, 0:128], sm["rs"][:, 0:1], cst[:, C_KVG:C_KVG + 128], ALU.mult, ALU.mult)
            cp("dve", widx_all[:, i, :], ps[:, 128:136])
            pt_ = PS(3584 + (i % 2) * 64, [128], BF16)
            tr(pt_, ckv_tok[:, i, 0:128], ident)
            cp("act", ckvT[:, ti], pt_)
        for sb in range(4):
            tsb = slice(sb * 512, (sb + 1) * 512)
            ps = PS(1024 + (sb % 2) * 512, [512])
            for kc in range(8):
                mm(ps, wki2[:, kc, :], hT[:, kc, tsb], start=(kc == 0), stop=(kc == 7))
            evac(kiT2[:, tsb], ps)

        slot = [0]

        def pslot(n):
            slot[0] = (slot[0] + 1) % 4
            return PS(slot[0] * 512, [n])

        for sbk in range(8):
            T0 = sbk * 256
            tq2 = slice(T0, T0 + 256)
            for cc in range(8):
                ps = pslot(256)
                for kc in range(8):
                    mm(ps, wq[:, kc, cc * 128:(cc + 1) * 128], hT[:, kc, tq2], start=(kc == 0), stop=(kc == 7))
                evac(qT_sb[:, cc, :], ps)
            for h in range(16):
                hb = (h % 2) * 64
                ps = pslot(256)
                mm(ps, wuk[hb:hb + 64, h // 2, :], qT_sb[hb:hb + 64, h // 2, :])
                ts("dve", qlatT[:, h, :], ps, 0.125, None, op0=ALU.mult)
            for hc in range(4):
                ps = pslot(256)
                for kc in range(8):
                    mm(ps, wqi[:, kc, hc * 128:(hc + 1) * 128], hT[:, kc, tq2], start=(kc == 0), stop=(kc == 7))
                evac(qiT[:, hc, :], ps)
            for j in range(2):
                i = sbk * 2 + j
                tq = slice(j * 128, (j + 1) * 128)
                Lk = (i + 1) * 128
                if i >= 2:
                    for hi_ in range(8):
                        hb = (hi_ % 2) * 64
                        for s0 in range(0, Lk, 512):
                            n = min(512, Lk - s0)
                            ps = pslot(n)
                            mm(ps, qiT[hb:hb + 64, hi_ // 2, tq], kiT2[hb:hb + 64, s0:s0 + n])
                            r_ = rl[(s0 // 512) % 2][:, 0:n]
                            act(r_, ps, AF.Relu)
                            wcol = widx_all[:, i, hi_:hi_ + 1]
                            if hi_ == 0:
                                ts("dve", score[:, s0:s0 + n], r_, wcol, None, op0=ALU.mult)
                            else:
                                stt(score[:, s0:s0 + n], r_, wcol, score[:, s0:s0 + n], ALU.mult, ALU.add)
                    tt("dve", score[:, i * 128:Lk], score[:, i * 128:Lk], cst[:, C_NTRI:C_NTRI + 128], ALU.add)
                    red(sm["lo"], score[:, 0:i * 128], ALU.min)
                    red(sm["hi"], score[:, 0:Lk], ALU.max)
                    tt("dve", sm["rng"], sm["hi"], sm["lo"], ALU.subtract)
                    ts("dve", steps, cst[:, C_PW:C_PW + NIT], sm["rng"][:, 0:1], None, op0=ALU.mult)
                    tt("dve", sm["cand"], sm["lo"], steps[:, 0:1], ALU.add)
                    for n_ in range(NIT):
                        ts("dve", maskq[:, 0:Lk], score[:, 0:Lk], sm["cand"][:, 0:1], None, op0=ALU.is_ge, op1=ALU.add, accum=sm["cnt"])
                        ts("dve", sm["gs"], sm["cnt"], 255.5, steps[:, n_:n_ + 1], op0=ALU.is_ge, op1=ALU.mult)
                        if n_ < NIT - 1:
                            stt(sm["cand"], sm["gs"], steps[:, n_ + 1:n_ + 2], sm["cand"], ALU.subtract, ALU.add)
                        else:
                            stt(sm["th"], sm["gs"], steps[:, n_:n_ + 1], sm["cand"], ALU.subtract, ALU.add)
                    ts("dve", maskq[:, 0:Lk], score[:, 0:Lk], sm["th"][:, 0:1], None, op0=ALU.is_ge)
                    for j0 in range(0, i + 1, 4):
                        nj = min(4, i + 1 - j0)
                        pm_ = PS(3584, [4, 128], BF16)
                        for jj in range(nj):
                            tr(pm_[:, jj, :], maskq[:, (j0 + jj) * 128:(j0 + jj + 1) * 128], ident)
                        evac(maskT[:, j0:j0 + nj, :], pm_[:, 0:nj, :])
                for hg in range(2):
                    for jc in range(i + 1):
                        par = jc % 2
                        lg = PS(par * 1024, [8, 128])
                        for a in range(2):
                            mm(lg[:, a * 4:(a + 1) * 4, :], ckvT[:, jc * 128:(jc + 1) * 128],
                               qlatT[:, hg * 8 + a * 4:hg * 8 + (a + 1) * 4, tq])
                        dl = i - jc
                        if dl <= 1:
                            tt("dve", tmpb, lg, btb[:, dl, hg * 8:(hg + 1) * 8, :], ALU.add)
                            act(Pm[par], tmpb, AF.Exp)
                        else:
                            act(Pm[par], lg, AF.Exp)
                        if i >= 2:
                            mk = maskT[:, jc, :]
                        elif jc == i:
                            mk = T1
                        else:
                            mk = None
                        if mk is not None:
                            tt("dve", PT[par], Pm[par], mk.unsqueeze(1).to_broadcast([128, 8, 128]), ALU.mult)
                            pt_use = PT[par]
                        else:
                            pt_use = Pm[par]
                        for h8 in range(8):
                            o_ = PS((4 + h8 // 3) * 512 + (h8 % 3) * 129, [129])
                            mm(o_, pt_use[:, h8, :], ckv_tok[:, jc, :], start=(jc == 0 and h8 % 3 == 0), stop=(jc == i),
                               skip_group_check=True)
                    for bi, (h0, h1) in enumerate(((0, 3), (3, 6), (6, 8))):
                        ob = PS((4 + bi) * 512, [h1 - h0, 129])
                        recip(rec[:, h0:h1], ob[:, :, 128])
                        tt("dve", att_lat[:, h0:h1, :], ob[:, :, 0:128],
                           rec[:, h0:h1].unsqueeze(2).to_broadcast([128, h1 - h0, 128]), ALU.mult)
                    pl = PS(3584, [8, 128], BF16)
                    for h8 in range(8):
                        tr(pl[:, h8, :], att_lat[:, h8, :], ident)
                    evac(att_latT[:, hg * 8:hg * 8 + 4, :], pl[:, 0:4, :])
                    evac(att_latT[:, hg * 8 + 4:hg * 8 + 8, :], pl[:, 4:8, :])
                aps = PS(0, [8, 128])
                for h in range(16):
                    hb = (h % 2) * 64
                    mm(aps[hb:hb + 64, h // 2, :], wuvT[:, h, :], att_latT[:, h, :])
                evac(attT_sb[:, :, tq], aps)
            for n in range(8):
                wload(wao[n % 2], D["wao"][n])
                wload(wga[n % 2], D["wga"][n])
                pa = PS(1024 + (n % 2) * 256, [256])
                pb_ = PS(1536 + (n % 2) * 256, [256])
                for kc in range(8):
                    mm(pa, wao[n % 2][:, kc, :], attT_sb[:, kc, :], start=(kc == 0), stop=(kc == 7))
                for kc in range(8):
                    mm(pb_, wga[n % 2][:, kc, :], hT[:, kc, tq2], start=(kc == 0), stop=(kc == 7))
                act(sga, pb_, AF.Sigmoid)
                tt("dve", tmpa, pa, sga, ALU.mult)
                tt("dve", mT[:, n, tq2], mT[:, n, tq2], tmpa, ALU.add)
        A.release(m_att)
        if dump("m2", mT.rearrange("p a b -> p (a b)"), 8 * S):
            return nc

        h2T = hT
        m_c1 = A.mark()
        wout = A.alloc([8, 1024], BF16)
        for kc in range(8):
            wload(wout[:, kc, :], D["wout"][:, kc, :])
        xts = [A.alloc([DM], F32) for _ in range(2)]
        x1s = [A.alloc([DM], F32) for _ in range(2)]
        scr = (A.alloc([DM], BF16), A.alloc([1], F32), A.alloc([1], F32), A.alloc([DM], BF16))
        for i in range(NT):
            ti = slice(i * 128, (i + 1) * 128)
            xt = xts[i % 2]
            x1 = x1s[i % 2]
            P.dma("sp", xt, xd[ti, :])
            ps = PS(2048 + (i % 2) * 1024, [1024])
            for hf in range(2):
                for kc in range(8):
                    mm(ps[:, hf * 512:(hf + 1) * 512], mT[:, kc, ti], wout[:, kc, hf * 512:(hf + 1) * 512],
                       start=(kc == 0), stop=(kc == 7))
            tt("dve", x1, xt, ps, ALU.add)
            P.dma("sp", x1d[ti, :], x1)
            norm_to_T(x1, C_GFFNB, h2T, i, scr)
        A.release(m_c1)

        prodT = A.alloc([22, S], BF16, top=True)
        m_c2 = A.mark()
        wup = [A.alloc([8, 256], BF16) for _ in range(2)]
        preG = A.alloc([1026], F32)
        preV = A.alloc([1026], F32)
        accG = A.alloc([1024], F32)
        accV = A.alloc([1024], F32)
        sgG = A.alloc([1024], F32)
        for c in range(22):
            wu = wup[c % 2]
            wload(wu, D["wup"][c])
            for hf in range(2):
                pg = PS(hf * 2048, [1024])
                pv = PS(hf * 2048 + 1024, [1024])
                for q in range(2):
                    tcs = slice(hf * 1024 + q * 512, hf * 1024 + (q + 1) * 512)
                    for kc in range(8):
                        mm(pg[:, q * 512:(q + 1) * 512], wu[:, kc, 0:128], h2T[:, kc, tcs], start=(kc == 0), stop=(kc == 7))
                    for kc in range(8):
                        mm(pv[:, q * 512:(q + 1) * 512], wu[:, kc, 128:256], h2T[:, kc, tcs], start=(kc == 0), stop=(kc == 7))
                for pre_, pp_ in ((preG, pg), (preV, pv)):
                    if hf == 0:
                        memset("pool", pre_[:, 0:2], 0.0)
                    else:
                        cp("pool", pre_[:, 0:2], pre_[:, 1024:1026])
                    cp("act", pre_[:, 2:1026], pp_)
                for pre_, acc_, cch in ((preG, accG, c), (preV, accV, 22 + c)):
                    w0 = C_CWF + cch * 3
                    ts("dve", acc_, pre_[:, 0:1024], cst[:, w0:w0 + 1], None, op0=ALU.mult)
                    for j in range(1, 3):
                        stt(acc_, pre_[:, j:j + 1024], cst[:, w0 + j:w0 + j + 1], acc_, ALU.mult, ALU.add)
                act(sgG, accG, AF.Silu, bias=cst[:, C_CBF + c:C_CBF + c + 1])
                stt(prodT[:, c, hf * 1024:(hf + 1) * 1024], accV, cst[:, C_CBF + 22 + c:C_CBF + 22 + c + 1], sgG, ALU.add, ALU.mult)
        A.release(m_c2)

        A.release(m_h)
        wdn = A.alloc([22, 1024], BF16)
        for c in range(22):
            wload(wdn[:, c, :], D["wdn"][:, c, :])
        x1s = [A.alloc([DM], F32) for _ in range(2)]
        x2 = A.alloc([DM], F32)
        ots = [A.alloc([DM], F32) for _ in range(2)]
        junk = A.alloc([DM], BF16)
        ssf = A.alloc([1], F32)
        rsf = A.alloc([1], F32)
        outs = []
        for i in range(NT):
            ti = slice(i * 128, (i + 1) * 128)
            x1 = x1s[i % 2]
            P.dma("sp", x1, x1d[ti, :])
            ps = PS((i % 2) * 1024, [1024])
            for hf in range(2):
                for c in range(22):
                    mm(ps[:, hf * 512:(hf + 1) * 512], prodT[:, c, ti], wdn[:, c, hf * 512:(hf + 1) * 512],
                       start=(c == 0), stop=(c == 21))
            tt("dve", x2, x1, ps, ALU.add)
            sumsq(junk, x2, ssf)
            rstd_pool(rsf, ssf, DM)
            stt(ots[i % 2], x2, rsf[:, 0:1], cst[:, C_GFIN:C_GFIN + 1024], ALU.mult, ALU.mult)
            outs.append(P.dma("sp", outd[ti, :], ots[i % 2]))
        P.wait_for("sp", outs)
        print("ops:", P.nops, {e: len(P.ops[e]) for e in ENGS}, "sbuf peak", A.peak)
        P.emit()
    return nc


def _run(inputs, ncores=8, debug=(), stop=None):
    shared = _prep_shared(inputs)
    x = np.asarray(inputs["x"], np.float32)
    nc = build(debug, stop)
    in_maps = [dict(shared, x=np.ascontiguousarray(x[b])) for b in range(ncores)]
    res = run_bass_kernel_spmd(nc, in_maps, core_ids=list(range(ncores)))
    return res


def kernel(**inputs):
    res = _run(inputs, 8)
    return np.stack([np.asarray(res.results[b]["out"]) for b in range(8)]).astype(np.float32)
```
